# Optimizing a Trainium2 kernel written in Bass

```python
import math
import jax
import jax.numpy as jnp
from jax import lax
import numpy as np


D_MODEL = 2048
BATCH = 8
SEQ = 4096
DEPTH = 4

CTX_LEN = 256
GRID_W = 64
N_MIXERS = 3
N_ML_LAYERS = (DEPTH + 2) // 3
N_WA_LAYERS = (DEPTH + 1) // 3
N_S5_LAYERS = DEPTH // 3
N_MOD = 9
D_FF = 5632
EPS = 1e-6
NEG_BIG = -1e30
ML_HEADS = 8
ML_HEAD_DIM = D_MODEL // ML_HEADS
ML_CHUNK = 64
WA_Q_HEADS = 16
WA_KV_HEADS = 4
WA_HEAD_DIM = D_MODEL // WA_Q_HEADS
WA_WINDOW = 128
WA_BLOCK = 128
ROPE_BASE = 10000.0
S5_GROUP = 16
S5_GROUPS = D_MODEL // S5_GROUP
S5_STATE = 64
S5_CHUNK = 128
S5_DT_MIN = 1e-3
S5_DT_MAX = 1e-1

kernel_name = 'hybrid_mlstm_swa_s5_macaron_dit'


def _rmsnorm(x, g):
    xf = x.astype(jnp.float32)
    y = xf * lax.rsqrt(jnp.mean(xf * xf, axis=-1, keepdims=True) + EPS)
    return (y * g.astype(jnp.float32)).astype(x.dtype)


def _adaln(x, g, shift, scale):
    return _rmsnorm(x, g) * (1 + scale) + shift


def _swiglu(h, w_in, w_out):
    a, gate = jnp.split(h @ w_in, 2, axis=-1)
    return (a * jax.nn.silu(gate)) @ w_out


def _flip(t, axis, rev):
    return jnp.flip(t, axis) if rev else t


def _axial_rope(n):
    rows = n // GRID_W
    row = jnp.repeat(jnp.arange(rows, dtype=jnp.float32), GRID_W)
    col = jnp.tile(jnp.arange(GRID_W, dtype=jnp.float32), rows)
    n_freq = WA_HEAD_DIM // 4
    inv = ROPE_BASE ** (-jnp.arange(n_freq, dtype=jnp.float32) / n_freq)
    ang = jnp.concatenate([row[:, None] * inv, col[:, None] * inv], axis=-1)
    return jnp.cos(ang), jnp.sin(ang)


def _rope(x, cos, sin):
    half = x.shape[-1] // 2
    shp = (1, x.shape[1]) + (1,) * (x.ndim - 3) + (half,)
    cs, sn = cos.reshape(shp), sin.reshape(shp)
    xf = x.astype(jnp.float32)
    x1, x2 = xf[..., :half], xf[..., half:]
    return jnp.concatenate([x1 * cs - x2 * sn, x2 * cs + x1 * sn], axis=-1).astype(x.dtype)


def _mlstm_scan(q, k, v, logi, logf, state, with_out):
    bsz, nh, length, _ = k.shape
    nc = length // ML_CHUNK

    def chunks(t):
        t = t.reshape(t.shape[:2] + (nc, ML_CHUNK) + t.shape[3:])
        return jnp.moveaxis(t, 2, 0)

    lower = jnp.tril(jnp.ones((ML_CHUNK, ML_CHUNK), dtype=bool))

    def body(carry, xs):
        c_mem, n_mem, m_run = carry
        qc, kc, vc, ic, fc = xs
        b = jnp.cumsum(fc, axis=-1)
        b_end = b[..., -1]
        g = b_end[..., None] - b + ic
        m_new = jnp.maximum(b_end + m_run, jnp.max(g, axis=-1))
        w = jnp.exp(g - m_new[..., None])
        dec = jnp.exp(b_end + m_run - m_new)
        c_new = dec[..., None, None] * c_mem + jnp.einsum('bhs,bhsd,bhsv->bhdv', w, kc, vc)
        n_new = dec[..., None] * n_mem + jnp.einsum('bhs,bhsd->bhd', w, kc)
        if not with_out:
            return (c_new, n_new, m_new), None
        d_log = jnp.where(lower, b[..., :, None] - b[..., None, :] + ic[..., None, :], NEG_BIG)
        m_prev = b + m_run[..., None]
        m_q = jnp.maximum(m_prev, jnp.max(d_log, axis=-1))
        s = jnp.einsum('bhld,bhsd->bhls', qc, kc) * jnp.exp(d_log - m_q[..., None])
        dq = jnp.exp(m_prev - m_q)
        num = jnp.einsum('bhls,bhsv->bhlv', s, vc) + dq[..., None] * jnp.einsum('bhld,bhdv->bhlv', qc, c_mem)
        den = jnp.sum(s, axis=-1) + dq * jnp.einsum('bhld,bhd->bhl', qc, n_mem)
        h = num / jnp.maximum(jnp.abs(den), jnp.exp(-m_q))[..., None]
        return (c_new, n_new, m_new), h

    xs = (chunks(q) if with_out else None, chunks(k), chunks(v), chunks(logi), chunks(logf))
    state, hs = lax.scan(body, state, xs)
    if not with_out:
        return None, state
    return jnp.moveaxis(hs, 0, 2).reshape(bsz, nh, length, -1), state


def _mlstm_mixer(h_lat, h_ctx, w_in, b_gate, g_head, w_out, with_ctx):
    dm = D_MODEL

    def heads(t):
        bsz, length, _ = t.shape
        return jnp.moveaxis(t.reshape(bsz, length, ML_HEADS, ML_HEAD_DIM), 2, 1).astype(jnp.float32)

    def gates(z):
        bsz, length, _ = z.shape
        gt = (z.astype(jnp.float32) + b_gate.astype(jnp.float32)).reshape(bsz, length, 4, ML_HEADS)
        gt = jnp.moveaxis(gt, 1, -1)
        return gt[:, 0], jax.nn.log_sigmoid(gt[:, 1]), gt[:, 2], jax.nn.log_sigmoid(gt[:, 3])

    def project(h, with_q):
        if with_q:
            q, k, v, o, g = jnp.split(h @ w_in, [dm, 2 * dm, 3 * dm, 4 * dm], axis=-1)
            q = heads(q) * ML_HEAD_DIM ** -0.5
        else:
            k, v = jnp.split(h @ w_in[:, dm:3 * dm], 2, axis=-1)
            g = h @ w_in[:, 4 * dm:]
            q, o = None, None
        return q, heads(k), heads(v), o, gates(g)

    ql, kl, vl, ol, (il_f, fl_f, il_b, fl_b) = project(h_lat, True)
    qc, kc, vc, oc, (ic_f, fc_f, ic_b, fc_b) = project(h_ctx, with_ctx)
    bsz = h_lat.shape[0]
    zero = (jnp.zeros((bsz, ML_HEADS, ML_HEAD_DIM, ML_HEAD_DIM), jnp.float32),
            jnp.zeros((bsz, ML_HEADS, ML_HEAD_DIM), jnp.float32),
            jnp.full((bsz, ML_HEADS), NEG_BIG, jnp.float32))

    def rev(t):
        return None if t is None else jnp.flip(t, axis=2)

    hc_f, st_f = _mlstm_scan(qc, kc, vc, ic_f, fc_f, zero, with_ctx)
    hc_b, st_b = _mlstm_scan(rev(qc), rev(kc), rev(vc), rev(ic_b), rev(fc_b), zero, with_ctx)
    hl_f, _ = _mlstm_scan(ql, kl, vl, il_f, fl_f, st_f, True)
    hl_b, _ = _mlstm_scan(rev(ql), rev(kl), rev(vl), rev(il_b), rev(fl_b), st_b, True)

    def merge(h_f, h_b, o):
        hs = h_f + rev(h_b)
        hs = hs * lax.rsqrt(jnp.mean(hs * hs, axis=-1, keepdims=True) + EPS)
        bsz_, _, length, _ = hs.shape
        hs = jnp.moveaxis(hs, 1, 2).reshape(bsz_, length, dm) * g_head.astype(jnp.float32)
        return (jax.nn.sigmoid(o.astype(jnp.float32)) * hs).astype(o.dtype) @ w_out

    y_ctx = merge(hc_f, hc_b, oc) if with_ctx else None
    return merge(hl_f, hl_b, ol), y_ctx


def _sink_softmax(s, sink):
    col = jnp.broadcast_to(sink[None, :, :, None, None], s.shape[:-1] + (1,))
    return jax.nn.softmax(jnp.concatenate([s, col], axis=-1), axis=-1)[..., :-1]


def _window_gqa_mixer(h_lat, h_ctx, w_in, sink, w_out, cos, sin, with_ctx):
    bsz, n, _ = h_lat.shape
    grp = WA_Q_HEADS // WA_KV_HEADS
    qd = WA_Q_HEADS * WA_HEAD_DIM
    kd = WA_KV_HEADS * WA_HEAD_DIM
    scale = WA_HEAD_DIM ** -0.5
    span = 3 * WA_BLOCK
    sink = sink.astype(jnp.float32).reshape(WA_KV_HEADS, grp)

    def split_q(t):
        return t.reshape(t.shape[0], t.shape[1], WA_KV_HEADS, grp, WA_HEAD_DIM)

    def split_kv(t):
        return t.reshape(t.shape[0], t.shape[1], WA_KV_HEADS, WA_HEAD_DIM)

    q, k, v = jnp.split(h_lat @ w_in, [qd, qd + kd], axis=-1)
    q = _rope(split_q(q), cos, sin)
    k = _rope(split_kv(k), cos, sin)
    v = split_kv(v)
    k_ctx, v_ctx = jnp.split(h_ctx @ w_in[:, qd:], 2, axis=-1)
    k_ctx, v_ctx = split_kv(k_ctx), split_kv(v_ctx)
    pad = ((0, 0), (WA_BLOCK, WA_BLOCK), (0, 0), (0, 0))
    k_pad, v_pad = jnp.pad(k, pad), jnp.pad(v, pad)

    def block(qb):
        start = qb * WA_BLOCK
        q_blk = lax.dynamic_slice_in_dim(q, start, WA_BLOCK, axis=1)
        k_blk = lax.dynamic_slice_in_dim(k_pad, start, span, axis=1)
        v_blk = lax.dynamic_slice_in_dim(v_pad, start, span, axis=1)
        q_pos = start + jnp.arange(WA_BLOCK)
        k_pos = start - WA_BLOCK + jnp.arange(span)
        ok = (jnp.abs(q_pos[:, None] - k_pos[None, :]) <= WA_WINDOW) & (k_pos >= 0) & (k_pos < n)
        s_loc = jnp.einsum('bqkgd,bskd->bkgqs', q_blk, k_blk, preferred_element_type=jnp.float32) * scale
        s_loc = jnp.where(ok, s_loc, NEG_BIG)
        s_ctx = jnp.einsum('bqkgd,bskd->bkgqs', q_blk, k_ctx, preferred_element_type=jnp.float32) * scale
        p = _sink_softmax(jnp.concatenate([s_loc, s_ctx], axis=-1), sink).astype(v.dtype)
        return (jnp.einsum('bkgqs,bskd->bqkgd', p[..., :span], v_blk)
                + jnp.einsum('bkgqs,bskd->bqkgd', p[..., span:], v_ctx))

    y = lax.map(block, jnp.arange(n // WA_BLOCK))
    y = jnp.moveaxis(y, 0, 1).reshape(bsz, n, D_MODEL) @ w_out
    y_ctx = None
    if with_ctx:
        q_c = split_q(h_ctx @ w_in[:, :qd])
        s = jnp.einsum('bqkgd,bskd->bkgqs', q_c, k_ctx, preferred_element_type=jnp.float32) * scale
        p = _sink_softmax(s, sink).astype(v_ctx.dtype)
        y_ctx = jnp.einsum('bkgqs,bskd->bqkgd', p, v_ctx).reshape(bsz, h_ctx.shape[1], D_MODEL) @ w_out
    return y, y_ctx


def _s5_discretize(lam_re, lam_im, log_dt, b_re, b_im):
    lam_re, lam_im = lam_re.astype(jnp.float32), lam_im.astype(jnp.float32)
    b_re, b_im = b_re.astype(jnp.float32), b_im.astype(jnp.float32)
    dt = jnp.exp(log_dt.astype(jnp.float32))[:, None]
    mag = jnp.exp(lam_re * dt)
    lb_re, lb_im = mag * jnp.cos(lam_im * dt), mag * jnp.sin(lam_im * dt)
    den = lam_re * lam_re + lam_im * lam_im
    nr, ni = lb_re - 1.0, lb_im
    fr = (nr * lam_re + ni * lam_im) / den
    fi = (ni * lam_re - nr * lam_im) / den
    bb_re = fr[..., None] * b_re - fi[..., None] * b_im
    bb_im = fr[..., None] * b_im + fi[..., None] * b_re
    return lb_re, lb_im, bb_re, bb_im


def _s5_scan(u, lb_re, lb_im, bb_re, bb_im, c_re, c_im, state, with_out):
    length, bsz = u.shape[:2]
    nc = length // S5_CHUNK
    a_re = jnp.broadcast_to(lb_re, (S5_CHUNK, bsz) + lb_re.shape)
    a_im = jnp.broadcast_to(lb_im, (S5_CHUNK, bsz) + lb_im.shape)

    def combine(e1, e2):
        a1r, a1i, b1r, b1i = e1
        a2r, a2i, b2r, b2i = e2
        return (a2r * a1r - a2i * a1i, a2r * a1i + a2i * a1r,
                a2r * b1r - a2i * b1i + b2r, a2r * b1i + a2i * b1r + b2i)

    def body(carry, uk):
        s_re, s_im = carry
        bu_re = jnp.einsum('lbgc,gpc->lbgp', uk, bb_re)
        bu_im = jnp.einsum('lbgc,gpc->lbgp', uk, bb_im)
        bu_re = bu_re.at[0].add(lb_re * s_re - lb_im * s_im)
        bu_im = bu_im.at[0].add(lb_re * s_im + lb_im * s_re)
        _, _, x_re, x_im = lax.associative_scan(combine, (a_re, a_im, bu_re, bu_im), axis=0)
        new = (x_re[-1], x_im[-1])
        if not with_out:
            return new, None
        y = jnp.einsum('lbgp,gcp->lbgc', x_re, c_re) - jnp.einsum('lbgp,gcp->lbgc', x_im, c_im)
        return new, y

    state, ys = lax.scan(body, state, u.reshape((nc, S5_CHUNK) + u.shape[1:]))
    if not with_out:
        return None, state
    return ys.reshape(u.shape), state


def _s5_mixer(h_lat, h_ctx, w_in, lam_re, lam_im, log_dt, b_re, b_im, c_re, c_im, d_skip, w_out, with_ctx):
    def to_groups(u):
        bsz_, length, _ = u.shape
        return jnp.moveaxis(u.astype(jnp.float32).reshape(bsz_, length, S5_GROUPS, S5_GROUP), 1, 0)

    u_lat = h_lat @ w_in
    u_ctx = h_ctx @ w_in
    t_lat, t_ctx = to_groups(u_lat), to_groups(u_ctx)
    bsz = h_lat.shape[0]
    zero = (jnp.zeros((bsz, S5_GROUPS, S5_STATE), jnp.float32),) * 2
    y_lat, y_ctx = [], []
    for d in range(2):
        rev = d == 1
        lb_re, lb_im, bb_re, bb_im = _s5_discretize(lam_re[d], lam_im[d], log_dt[d], b_re[d], b_im[d])
        cr, ci = c_re[d].astype(jnp.float32), c_im[d].astype(jnp.float32)
        yc, s_end = _s5_scan(_flip(t_ctx, 0, rev), lb_re, lb_im, bb_re, bb_im, cr, ci, zero, with_ctx)
        yl, _ = _s5_scan(_flip(t_lat, 0, rev), lb_re, lb_im, bb_re, bb_im, cr, ci, s_end, True)
        y_lat.append(_flip(yl, 0, rev))
        if with_ctx:
            y_ctx.append(_flip(yc, 0, rev))

    def glu_out(ys, u):
        bsz_, length, _ = u.shape
        y = jnp.moveaxis(ys[0] + ys[1], 0, 1).reshape(bsz_, length, D_MODEL)
        y = y + d_skip.astype(jnp.float32) * u.astype(jnp.float32)
        z = jax.nn.gelu(y).astype(u.dtype)
        a, gate = jnp.split(z @ w_out, 2, axis=-1)
        return a * jax.nn.sigmoid(gate)

    return glu_out(y_lat, u_lat), (glu_out(y_ctx, u_ctx) if with_ctx else None)


def setup_inputs(seed: int = 0) -> dict:
    key = jax.random.key(seed)
    ks = list(jax.random.split(key, 32))

    def nrm(i, shape, std):
        return std * jax.random.normal(ks[i], shape, jnp.float32)

    dm = D_MODEL
    f_bias = jnp.linspace(3.0, 6.0, ML_HEADS, dtype=jnp.float32)
    ml_b_gate = jnp.concatenate([
        nrm(12, (N_ML_LAYERS, ML_HEADS), 0.1),
        f_bias + nrm(13, (N_ML_LAYERS, ML_HEADS), 0.1),
        nrm(14, (N_ML_LAYERS, ML_HEADS), 0.1),
        f_bias + nrm(15, (N_ML_LAYERS, ML_HEADS), 0.1)], axis=-1)
    s5_shape = (N_S5_LAYERS, 2, S5_GROUPS, S5_STATE)
    state_idx = jnp.arange(S5_STATE, dtype=jnp.float32)
    return {
        'x': nrm(0, (BATCH, SEQ, dm), 1.0),
        'c': nrm(1, (BATCH, dm), 1.0),
        'ctx': nrm(2, (BATCH, CTX_LEN, dm), 1.0),
        'c_ctx': nrm(3, (dm,), 1.0),
        'w_ada': nrm(4, (DEPTH, dm, N_MOD * dm), 0.5 * dm ** -0.5),
        'b_ada': nrm(5, (DEPTH, N_MOD * dm), 0.02),
        'g_norm': 1.0 + nrm(6, (DEPTH, 3, dm), 0.02),
        'w_ffn_in': nrm(7, (DEPTH, 2, dm, 2 * D_FF), dm ** -0.5),
        'w_ffn_out': nrm(8, (DEPTH, 2, D_FF, dm), D_FF ** -0.5),
        'g_final': 1.0 + nrm(9, (dm,), 0.02),
        'ml_w_in': nrm(10, (N_ML_LAYERS, dm, 4 * dm + 4 * ML_HEADS), dm ** -0.5),
        'ml_b_gate': ml_b_gate,
        'ml_g_head': 1.0 + nrm(11, (N_ML_LAYERS, dm), 0.02),
        'ml_w_out': nrm(16, (N_ML_LAYERS, dm, dm), dm ** -0.5),
        'wa_w_in': nrm(17, (N_WA_LAYERS, dm, (WA_Q_HEADS + 2 * WA_KV_HEADS) * WA_HEAD_DIM), dm ** -0.5),
        'wa_sink': nrm(18, (N_WA_LAYERS, WA_Q_HEADS), 0.5),
        'wa_w_out': nrm(19, (N_WA_LAYERS, dm, dm), dm ** -0.5),
        's5_w_in': nrm(20, (N_S5_LAYERS, dm, dm), dm ** -0.5),
        's5_lam_re': -0.5 + nrm(21, s5_shape, 0.01),
        's5_lam_im': math.pi * state_idx + nrm(22, s5_shape, 0.01),
        's5_log_dt': jax.random.uniform(ks[23], (N_S5_LAYERS, 2, S5_GROUPS), jnp.float32,
                                        math.log(S5_DT_MIN), math.log(S5_DT_MAX)),
        's5_b_re': nrm(24, s5_shape + (S5_GROUP,), (2 * S5_GROUP) ** -0.5),
        's5_b_im': nrm(25, s5_shape + (S5_GROUP,), (2 * S5_GROUP) ** -0.5),
        's5_c_re': nrm(26, (N_S5_LAYERS, 2, S5_GROUPS, S5_GROUP, S5_STATE), 0.5),
        's5_c_im': nrm(27, (N_S5_LAYERS, 2, S5_GROUPS, S5_GROUP, S5_STATE), 0.5),
        's5_d_skip': nrm(28, (N_S5_LAYERS, dm), 0.5),
        's5_w_out': nrm(29, (N_S5_LAYERS, dm, 2 * dm), dm ** -0.5),
    }


def reference(x, c, ctx, c_ctx, w_ada, b_ada, g_norm, w_ffn_in, w_ffn_out, g_final,
              ml_w_in, ml_b_gate, ml_g_head, ml_w_out,
              wa_w_in, wa_sink, wa_w_out,
              s5_w_in, s5_lam_re, s5_lam_im, s5_log_dt, s5_b_re, s5_b_im, s5_c_re, s5_c_im,
              s5_d_skip, s5_w_out):
    bsz, n, dm = x.shape
    cos, sin = _axial_rope(n)
    s_c = jax.nn.silu(c)
    s_cc = jax.nn.silu(c_ctx)
    h, hc = x, ctx
    for layer in range(DEPTH):
        has_next = layer < DEPTH - 1
        mod = (s_c @ w_ada[layer] + b_ada[layer]).reshape(bsz, N_MOD, 1, dm)
        mod_c = (s_cc @ w_ada[layer] + b_ada[layer]).reshape(N_MOD, dm)
        g = g_norm[layer]
        h = h + 0.5 * mod[:, 2] * _swiglu(_adaln(h, g[0], mod[:, 0], mod[:, 1]), w_ffn_in[layer, 0], w_ffn_out[layer, 0])
        hc = hc + 0.5 * mod_c[2] * _swiglu(_adaln(hc, g[0], mod_c[0], mod_c[1]), w_ffn_in[layer, 0], w_ffn_out[layer, 0])
        a_l = _adaln(h, g[1], mod[:, 3], mod[:, 4])
        a_c = _adaln(hc, g[1], mod_c[3], mod_c[4])
        kind, idx = layer % N_MIXERS, layer // N_MIXERS
        if kind == 0:
            y, y_c = _mlstm_mixer(a_l, a_c, ml_w_in[idx], ml_b_gate[idx], ml_g_head[idx], ml_w_out[idx], has_next)
        elif kind == 1:
            y, y_c = _window_gqa_mixer(a_l, a_c, wa_w_in[idx], wa_sink[idx], wa_w_out[idx], cos, sin, has_next)
        else:
            y, y_c = _s5_mixer(a_l, a_c, s5_w_in[idx], s5_lam_re[idx], s5_lam_im[idx], s5_log_dt[idx],
                               s5_b_re[idx], s5_b_im[idx], s5_c_re[idx], s5_c_im[idx],
                               s5_d_skip[idx], s5_w_out[idx], has_next)
        h = h + mod[:, 5] * y
        h = h + 0.5 * mod[:, 8] * _swiglu(_adaln(h, g[2], mod[:, 6], mod[:, 7]), w_ffn_in[layer, 1], w_ffn_out[layer, 1])
        if has_next:
            hc = hc + mod_c[5] * y_c
            hc = hc + 0.5 * mod_c[8] * _swiglu(_adaln(hc, g[2], mod_c[6], mod_c[7]), w_ffn_in[layer, 1], w_ffn_out[layer, 1])
    return _rmsnorm(h, g_final)
```

```python
import contextlib
import math
import numpy as np
import concourse.bass as bass
import concourse.mybir as mybir
from concourse.bass_utils import run_bass_kernel_spmd

F32 = mybir.dt.float32
BF16 = mybir.dt.bfloat16
AF = mybir.ActivationFunctionType
ALU = mybir.AluOpType
AX = mybir.AxisListType

D = 2048
NCH = D // 128
N_MOD = 9
EPS = 1e-6


class Cfg:
    def __init__(self, SEQ=4096, CTX=256, DFF=5632, DEPTH=4, B=8, stop_after=None, dbg=99):
        self.SEQ, self.CTX, self.DFF, self.DEPTH, self.B = SEQ, CTX, DFF, DEPTH, B
        self.NT = SEQ + CTX
        self.stop_after = stop_after
        self.dbg = dbg


class Buf:
    __slots__ = ("w", "r", "name", "excl")

    def __init__(self, name="", excl=False):
        self.w = None
        self.r = {}
        self.name = name
        self.excl = excl


def bufs(n, name=""):
    return [Buf(f"{name}{i}") for i in range(n)]


class Eng:
    def __init__(self, name, b):
        self.name, self.b = name, b
        self.key = None
        self.count = 0
        self.waited = {}
        self.dma_keys = []
        self.dma_count = 0


def _flat(x):
    if x is None:
        return []
    if isinstance(x, Buf):
        return [x]
    out = []
    for y in x:
        out.extend(_flat(y))
    return out


class KB:
    NDMA = 12

    def __init__(self, nc, es):
        self.nc, self.es = nc, es
        self.sems = []
        self.eng = {
            "pe": Eng("pe", nc.tensor), "act": Eng("act", nc.scalar), "dve": Eng("dve", nc.vector),
            "pool": Eng("pool", nc.gpsimd), "sp": Eng("sp", nc.sync),
        }
        for n, e in self.eng.items():
            e.key = self._newsem(f"s_{n}")
        for n in ("sp", "pool", "act"):
            e = self.eng[n]
            e.dma_keys = [self._newsem(f"d_{n}{i}") for i in range(self.NDMA)]
        self.semval = {}
        self.uid = 0

    def _newsem(self, name):
        s = self.es.enter_context(self.nc.semaphore(name))
        self.sems.append(s)
        return len(self.sems) - 1

    def _wait(self, E, deps):
        best = {}
        for d in deps:
            if d is None:
                continue
            k, v = d
            if best.get(k, 0) < v:
                best[k] = v
        for k, v in best.items():
            if k == E.key and E.name == "pe":
                continue
            if E.waited.get(k, 0) < v:
                E.b.wait_ge(self.sems[k], v)
                E.waited[k] = v

    def _deps(self, r, w):
        deps = []
        for b in r:
            deps.append(b.w)
        for b in w:
            deps.append(b.w)
            deps.extend(b.r.items())
        return deps

    def _mark(self, tok, r, w):
        k, v = tok
        for b in r:
            if b.r.get(k, 0) < v:
                b.r[k] = v
        for b in w:
            b.w = tok
            b.r = {}
        self.semval[k] = v

    def op(self, eng, fn, r=(), w=()):
        E = self.eng[eng]
        r, w = _flat(r), _flat(w)
        w = w + [b for b in r if b.excl]
        r = [b for b in r if not b.excl]
        self._wait(E, self._deps(r, w))
        ins = fn(E.b)
        E.count += 1
        ins.then_inc(self.sems[E.key], 1)
        self._mark((E.key, E.count), r, w)

    def dma(self, q, out, in_, r=(), w=()):
        E = self.eng[q]
        r, w = _flat(r), _flat(w)
        j = E.dma_count
        slot = j % self.NDMA
        deps = self._deps(r, w)
        if j >= self.NDMA:
            deps.append((E.dma_keys[slot], 16 * (j // self.NDMA)))
        self._wait(E, deps)
        E.b.dma_start(out=out, in_=in_).then_inc(self.sems[E.dma_keys[slot]], 16)
        E.dma_count += 1
        self._mark((E.dma_keys[slot], 16 * (j // self.NDMA + 1)), r, w)

    def barrier(self):
        allv = list(self.semval.items())
        for E in self.eng.values():
            for k, v in allv:
                if k == E.key:
                    continue
                if E.waited.get(k, 0) < v:
                    E.b.wait_ge(self.sems[k], v)
                    E.waited[k] = v

    def name(self, p):
        self.uid += 1
        return f"{p}_{self.uid}"


class Prog:
    def __init__(self, cfg):
        self.cfg = cfg
        self.nc = bass.Bass("TRN2", target_bir_lowering=False)
        self.es = contextlib.ExitStack()
        self.kb = KB(self.nc, self.es)
        self.din = {}
        self.psum = []
        self.ps_i = 0

    def dram_in(self, name, shape, dtype=F32):
        t = self.nc.dram_tensor(name, list(shape), dtype, kind="ExternalInput")
        self.din[name] = t
        return t.ap()

    def dram_scratch(self, name, shape, dtype=F32):
        return self.nc.dram_tensor(name, list(shape), dtype, kind="Internal").ap()

    def sb(self, st, name, shape, dtype=F32):
        return st.enter_context(self.nc.sbuf_tensor(self.kb.name(name), list(shape), dtype))

    def wget(self, key, view, bW, loader):
        kb = self.kb
        n = 1
        for d in view.shape[1:]:
            n *= d
        if key in self.wc:
            pg, off, buf = self.wc[key]
            src = self.ws_pages[pg][:, off:off + n]
            if len(view.shape) == 3:
                src = src.rearrange("p (k n) -> p k n", k=view.shape[1])
            kb.dma("sp", view, src, r=[buf], w=[bW])
        else:
            loader()
            if not self.ws_pages or self.ws_off + n > self.PAGE_COLS:
                self.ws_pages.append(self.dram_scratch(f"WS{len(self.ws_pages)}", [128, self.PAGE_COLS], BF16))
                self.ws_off = 0
            pg = len(self.ws_pages) - 1
            off = self.ws_off
            self.ws_off += n
            buf = Buf("ws_" + key)
            dst = self.ws_pages[pg][:, off:off + n]
            if len(view.shape) == 3:
                dst = dst.rearrange("p (k n) -> p k n", k=view.shape[1])
            kb.dma("sp", dst, view, r=[bW], w=[buf])
            self.wc[key] = (pg, off, buf)

    def ps(self):
        t, b = self.psum[self.ps_i % len(self.psum)]
        self.ps_i += 1
        return t, b

    def mm_group(self, out_ap, pairs, r, w):
        n = len(pairs)

        def fn(e):
            ins = None
            for i, (l, rr) in enumerate(pairs):
                ins = e.matmul(out_ap, l, rr, start=(i == 0), stop=(i == n - 1))
            return ins
        self.kb.op("pe", fn, r=r, w=w)

    def transpose(self, out_ap, in_ap, r, w):
        np_ = in_ap.shape[0]
        ident = self.ident
        self.kb.op("pe", lambda e: e.transpose(out_ap, in_ap, ident[0:np_, 0:np_]), r=r, w=w)


def build(cfg):
    P = Prog(cfg)
    nc, kb = P.nc, P.kb
    SEQ, CTX, DFF, NT = cfg.SEQ, cfg.CTX, cfg.DFF, cfg.NT
    L = cfg.DEPTH
    KF = DFF // 128

    x_in = P.dram_in("x", [SEQ, D])
    c_in = P.dram_in("c", [NCH, 128])
    ctx_in = P.dram_in("ctx", [CTX, D])
    cctx_in = P.dram_in("c_ctx", [NCH, 128])
    w_ada = P.dram_in("w_ada", [L, D, N_MOD * D])
    b_ada = P.dram_in("b_ada", [L, N_MOD * NCH, 128])
    g_norm = P.dram_in("g_norm", [L, 3 * NCH, 128])
    w_ffn_in = P.dram_in("w_ffn_in", [L, 2, D, 2 * DFF])
    w_ffn_out = P.dram_in("w_ffn_out", [L, 2, DFF, D])
    g_final = P.dram_in("g_final", [NCH, 128])
    ident_in = P.dram_in("ident", [128, 128])
    tri_in = P.dram_in("tri", [3, 128, 128])
    NML, NWA, NS5 = (L + 2) // 3, (L + 1) // 3, L // 3
    ml_w_in = P.dram_in("ml_w_in", [NML, D, 4 * D + 32])
    ml_b_gate = P.dram_in("ml_b_gate", [NML, 32])
    ml_g_head = P.dram_in("ml_g_head", [NML, D])
    ml_w_out = P.dram_in("ml_w_out", [NML, D, D])
    HD = [P.dram_scratch(f"HD{i}", [NT, D]) for i in range(2)]
    QTd = P.dram_scratch("QTd", [D, NT], BF16)
    KTd = P.dram_scratch("KTd", [D, NT], BF16)
    KKd = P.dram_scratch("KKd", [NT, D], BF16)
    VRd = P.dram_scratch("VRd", [NT, D], BF16)
    Zd = P.dram_scratch("Zd", [NT, 32])
    if NS5 > 0:
        s5_w_in = P.dram_in("s5_w_in", [NS5, D, D])
        s5_lam_re = P.dram_in("s5_lam_re", [NS5, 2, 128, 64])
        s5_lam_im = P.dram_in("s5_lam_im", [NS5, 2, 128, 64])
        s5_log_dt = P.dram_in("s5_log_dt", [NS5, 2, 128])
        s5_b_re = P.dram_in("s5_b_re", [NS5, 2, 128, 64, 16])
        s5_b_im = P.dram_in("s5_b_im", [NS5, 2, 128, 64, 16])
        s5_c_re = P.dram_in("s5_c_re", [NS5, 2, 128, 16, 64])
        s5_c_im = P.dram_in("s5_c_im", [NS5, 2, 128, 16, 64])
        s5_d_skip = P.dram_in("s5_d_skip", [NS5, NCH, 128])
        s5_w_out = P.dram_in("s5_w_out", [NS5, D, 2 * D])
        iota_in = P.dram_in("iota1", [128, 512])
        UTd = P.dram_scratch("UTd", [D, NT])
        ZTd = P.dram_scratch("ZTd", [D, NT], BF16)
    if NWA > 0:
        wa_w_in = P.dram_in("wa_w_in", [NWA, D, 3072])
        wa_sink = P.dram_in("wa_sink", [NWA, 16])
        wa_w_out = P.dram_in("wa_w_out", [NWA, D, D])
        rope_cos = P.dram_in("rope_cos", [128, SEQ])
        rope_sin = P.dram_in("rope_sin", [128, SEQ])
    out = nc.dram_tensor("out", [SEQ, D], F32, kind="ExternalOutput").ap()
    HT = P.dram_scratch("HT", [D, NT])
    P.PAGE_COLS = 524288
    P.ws_pages, P.wc, P.ws_off = [], {}, 0
    HTv = HT.rearrange("(c p) t -> p c t", p=128)
    ht_bufs = {}

    def htb(t0):
        return ht_bufs.setdefault(t0, Buf(f"ht{t0}"))

    tiles = []
    for t0 in range(0, CTX, 512):
        tiles.append((t0, min(512, CTX - t0), 1))
    for t0 in range(0, SEQ, 512):
        tiles.append((CTX + t0, min(512, SEQ - t0), 0))

    for i in range(8):
        t = P.es.enter_context(nc.psum_tensor(f"psb{i}", [128, 512], F32))
        P.psum.append((t, Buf(f"ps{i}", excl=True)))

    pst = P.es
    ident = P.sb(pst, "ident", [128, 128]); b_ident = Buf("ident")
    P.ident = ident
    ones_bf = P.sb(pst, "ones", [128, 128], BF16); b_ones = Buf("ones")
    sT = P.sb(pst, "sT", [128, NCH, 2], BF16); b_sT = Buf("sT")
    modT = P.sb(pst, "modT", [128, N_MOD, NCH, 2]); b_mod = Buf("modT")
    gT = P.sb(pst, "gT", [128, 3, NCH]); b_gT = Buf("gT")
    Asc = P.sb(pst, "Asc", [128, 6, NCH]); b_A = Buf("Asc")
    Gt = P.sb(pst, "Gt", [128, 6, NCH]); b_G = Buf("Gt")
    gfin = P.sb(pst, "gfin", [128, NCH]); b_gfin = Buf("gfin")

    kb.dma("sp", ident[:], ident_in[:, :], w=[b_ident])
    tri = P.sb(pst, "tri", [128, 3, 128]); b_tri = Buf("tri")
    for i in range(3):
        kb.dma("sp", tri[:, i, :], tri_in[i, :, :], w=[b_tri])
    kb.op("dve", lambda e: e.memset(ones_bf[:], 1.0), w=[b_ones])

    with contextlib.ExitStack() as st:
        xin = [P.sb(st, "xin", [128, D]) for _ in range(2)]; b_xin = bufs(2, "xin")
        xo = [P.sb(st, "xo", [128, NCH, 128]) for _ in range(2)]; b_xo = bufs(2, "xo")
        cc = P.sb(st, "cc", [32, 128]); b_cc = Buf("cc")
        cs = P.sb(st, "cs", [32, 128]); b_cs = Buf("cs")
        gf = P.sb(st, "gf", [NCH, 128]); b_gf = Buf("gf")
        kb.dma("sp", cc[0:NCH, :], c_in[:, :], w=[b_cc])
        kb.dma("sp", cc[NCH:2 * NCH, :], cctx_in[:, :], w=[b_cc])
        kb.op("act", lambda e: e.activation(out=cs[:], in_=cc[:], func=AF.Silu), r=[b_cc], w=[b_cs])
        pt, pb = P.ps()
        P.transpose(pt[:, 0:32], cs[:, :], r=[b_cs, b_ident], w=[pb])
        kb.op("dve", lambda e: e.tensor_copy(out=sT[:].rearrange("p k s -> p s k"),
                                             in_=pt[:, 0:32].rearrange("p (s k) -> p s k", s=2)),
              r=[pb], w=[b_sT])
        kb.dma("sp", gf[:], g_final[:, :], w=[b_gf])
        pt, pb = P.ps()
        P.transpose(pt[:, 0:NCH], gf[:, :], r=[b_gf, b_ident], w=[pb])
        kb.op("dve", lambda e: e.tensor_copy(out=gfin[:], in_=pt[:, 0:NCH]), r=[pb], w=[b_gfin])
        blocks = [(ctx_in, r0, r0) for r0 in range(0, CTX, 128)] + [(x_in, r0, CTX + r0) for r0 in range(0, SEQ, 128)]
        for i, (src, r0, t0) in enumerate(blocks):
            s = i % 2
            kb.dma("sp", xin[s][:], src[r0:r0 + 128, :], w=[b_xin[s]])
            for q in range(4):
                pt, pb = P.ps()
                for c4 in range(4):
                    c = q * 4 + c4
                    P.transpose(pt[:, c4 * 128:(c4 + 1) * 128], xin[s][:, c * 128:(c + 1) * 128],
                                r=[b_xin[s], b_ident], w=[pb])
                eng = "act" if q % 2 else "dve"
                if eng == "act":
                    kb.op("act", lambda e: e.copy(out=xo[s][:, q * 4:(q + 1) * 4, :],
                                                  in_=pt[:].rearrange("p (c t) -> p c t", c=4)),
                          r=[pb], w=[b_xo[s]])
                else:
                    kb.op("dve", lambda e: e.tensor_copy(out=xo[s][:, q * 4:(q + 1) * 4, :],
                                                         in_=pt[:].rearrange("p (c t) -> p c t", c=4)),
                          r=[pb], w=[b_xo[s]])
            kb.dma("sp", HTv[:, :, t0:t0 + 128], xo[s][:], r=[b_xo[s]], w=[htb(t0)])
    kb.barrier()

    def tile_buf(t0):
        return htb(t0)

    def load_x(X, bX, t0, T):
        kb.dma("sp", X[:, :, 0:T], HTv[:, :, t0:t0 + T], r=[tile_buf(t0)], w=[bX])

    def norm_tile(st_tmp, X, bX, XN, bXN, T, A_ap, B_ap, out_f32=False):
        tmp, b_tmp, rstd, b_rstd, sq, b_sq = st_tmp
        if sq is None:
            sq, b_sq = XN, bXN
        kb.op("act", lambda e: e.activation(out=sq[:, :, 0:T], in_=X[:, :, 0:T], func=AF.Square), r=[bX], w=[b_sq])
        pt, pb = P.ps()
        P.mm_group(pt[:, 0:T], [(ones_bf[:, :], sq[:, c, 0:T]) for c in range(NCH)], r=[b_sq, b_ones], w=[pb])
        kb.op("dve", lambda e: e.tensor_scalar(out=rstd[:, 0:T], in0=pt[:, 0:T], scalar1=1.0 / D, scalar2=EPS,
                                               op0=ALU.mult, op1=ALU.add), r=[pb], w=[b_rstd])
        kb.op("act", lambda e: e.activation(out=rstd[:, 0:T], in_=rstd[:, 0:T], func=AF.Sqrt), r=[b_rstd], w=[b_rstd])
        kb.op("dve", lambda e: e.reciprocal(out=rstd[:, 0:T], in_=rstd[:, 0:T]), r=[b_rstd], w=[b_rstd])
        for c in range(NCH):
            i = c % 2
            if B_ap is None:
                kb.op("dve", lambda e: e.scalar_tensor_tensor(out=XN[:, c, 0:T], in0=X[:, c, 0:T], scalar=A_ap[:, c:c + 1],
                                                              in1=rstd[:, 0:T], op0=ALU.mult, op1=ALU.mult),
                      r=[bX, b_rstd], w=[bXN])
            else:
                kb.op("dve", lambda e: e.scalar_tensor_tensor(out=tmp[i][:, 0:T], in0=X[:, c, 0:T], scalar=A_ap[:, c:c + 1],
                                                              in1=rstd[:, 0:T], op0=ALU.mult, op1=ALU.mult),
                      r=[bX, b_rstd], w=[b_tmp[i]])
                kb.op("act", lambda e: e.activation(out=XN[:, c, 0:T], in_=tmp[i][:, 0:T], func=AF.Identity,
                                                    bias=B_ap[:, c:c + 1], scale=1.0),
                      r=[b_tmp[i]], w=[bXN])

    def alloc_norm_tmp(st, Tmax=512, need_sq=False):
        tmp = [P.sb(st, "ntmp", [128, Tmax]) for _ in range(2)]
        rstd = P.sb(st, "rstd", [128, Tmax])
        sq = P.sb(st, "sq", [128, NCH, Tmax], BF16) if need_sq else None
        return (tmp, bufs(2, "ntmp"), rstd, Buf("rstd"), sq, Buf("sq"))

    def mods_phase(layer):
        with contextlib.ExitStack() as st:
            NWA_ = 6
            wt = [P.sb(st, "wada", [128, NCH, 512], BF16) for _ in range(NWA_)]; b_wt = bufs(NWA_, "wada")
            brow = P.sb(st, "brow", [128, 2, 128]); b_brow = Buf("brow")
            bT = P.sb(st, "bT", [128, N_MOD * NCH]); b_bT = Buf("bT")
            grow = P.sb(st, "grow", [3 * NCH, 128]); b_grow = Buf("grow")
            wv = w_ada[layer].rearrange("(kc p) n -> p kc n", p=128)
            pm, pmb = P.ps()
            nblk = N_MOD * NCH
            for cb in range(nblk // 4):
                s = cb % NWA_
                kb.dma("pool", wt[s][:], wv[:, :, cb * 512:(cb + 1) * 512], w=[b_wt[s]])
                for j in range(4):
                    blk = cb * 4 + j
                    P.mm_group(pm[:, blk * 2:(blk + 1) * 2],
                               [(wt[s][:, kc, j * 128:(j + 1) * 128], sT[:, kc, :]) for kc in range(NCH)],
                               r=[b_wt[s], b_sT], w=[pmb])
            kb.dma("sp", brow[:, 0, :], b_ada[layer, 0:128, :], w=[b_brow])
            kb.dma("sp", brow[0:nblk - 128, 1, :], b_ada[layer, 128:nblk, :], w=[b_brow])
            pt, pb = P.ps()
            P.transpose(pt[:, 0:128], brow[:, 0, :], r=[b_brow, b_ident], w=[pb])
            P.transpose(pt[:, 128:nblk], brow[0:nblk - 128, 1, :], r=[b_brow, b_ident], w=[pb])
            kb.op("dve", lambda e: e.tensor_copy(out=bT[:], in_=pt[:, 0:nblk]), r=[pb], w=[b_bT])
            kb.op("dve", lambda e: e.tensor_tensor(
                out=modT[:].rearrange("p v c s -> p (v c) s"),
                in0=pm[:, 0:2 * nblk].rearrange("p (b s) -> p b s", s=2),
                in1=bT[:].unsqueeze(2).to_broadcast([128, nblk, 2]), op=ALU.add),
                r=[pmb, b_bT], w=[b_mod])
            kb.dma("sp", grow[:], g_norm[layer, :, :], w=[b_grow])
            pt, pb = P.ps()
            P.transpose(pt[:, 0:3 * NCH], grow[:, :], r=[b_grow, b_ident], w=[pb])
            kb.op("dve", lambda e: e.tensor_copy(out=gT[:].rearrange("p j c -> p (j c)"), in_=pt[:, 0:3 * NCH]),
                  r=[pb], w=[b_gT])
            for j in range(3):
                for s in range(2):
                    kb.op("dve", lambda e: e.scalar_tensor_tensor(
                        out=Asc[:, j * 2 + s, :], in0=modT[:, 3 * j + 1, :, s], scalar=1.0, in1=gT[:, j, :],
                        op0=ALU.add, op1=ALU.mult), r=[b_mod, b_gT], w=[b_A])
                    kb.op("dve", lambda e: e.tensor_scalar(
                        out=Gt[:, j * 2 + s, :], in0=modT[:, 3 * j + 2, :, s], scalar1=(1.0 if j == 1 else 0.5),
                        scalar2=None, op0=ALU.mult), r=[b_mod], w=[b_G])
        kb.barrier()

    def ffn_phase(layer, half, with_ctx):
        jn = 0 if half == 0 else 2
        with contextlib.ExitStack() as st:
            X = P.sb(st, "X", [128, NCH, 512]); bX = Buf("X")
            XNs = [P.sb(st, "XN", [128, NCH, 512], BF16) for _ in range(2)]; bXNs = bufs(2, "XN")
            AT = P.sb(st, "AT", [128, KF, 512], BF16); b_AT = bufs(KF, "AT")
            NW = 4
            W = [P.sb(st, "W", [128, 8192], BF16) for _ in range(NW)]; b_W = bufs(NW, "W")
            SG = [P.sb(st, "SG", [128, 512]) for _ in range(2)]; b_SG = bufs(2, "SG")
            NR = 6
            XRg = [P.sb(st, "XRg", [128, 512]) for _ in range(NR)]; b_XRg = bufs(NR, "XRg")
            ntmp = alloc_norm_tmp(st)
            wi = w_ffn_in[layer, half].rearrange("(kc p) n -> p kc n", p=128)
            wo = w_ffn_out[layer, half].rearrange("(kc p) n -> p kc n", p=128)
            wcnt = 0
            rcnt = 0
            lat_tiles = [t for t in tiles if t[2] == 0]
            ctx_tiles = [t for t in tiles if t[2] == 1] if with_ctx else []
            order = lat_tiles[:1] + ctx_tiles + lat_tiles[1:]

            def prep(i):
                t0, T, s = order[i]
                load_x(X, bX, t0, T)
                norm_tile(ntmp, X, bX, XNs[i % 2], bXNs[i % 2], T, Asc[:, jn * 2 + s, :], modT[:, 3 * jn, :, s])

            prep(0)
            for i, (t0, T, s) in enumerate(order):
                XN, bXN = XNs[i % 2], bXNs[i % 2]
                for jb in range(DFF // 512):
                    ia, ig = wcnt % NW, (wcnt + 1) % NW
                    wcnt += 2
                    Wa = W[ia][:].rearrange("p (k n) -> p k n", k=NCH)
                    Wg = W[ig][:].rearrange("p (k n) -> p k n", k=NCH)
                    P.wget(f"fi{layer}_{half}_a{jb}", Wa, b_W[ia],
                           lambda: kb.dma("pool", Wa, wi[:, :, jb * 512:(jb + 1) * 512], w=[b_W[ia]]))
                    P.wget(f"fi{layer}_{half}_g{jb}", Wg, b_W[ig],
                           lambda: kb.dma("pool", Wg, wi[:, :, DFF + jb * 512:DFF + (jb + 1) * 512], w=[b_W[ig]]))
                    for jj in range(4):
                        fc = jb * 4 + jj
                        pa, pab = P.ps()
                        pg, pgb = P.ps()
                        P.mm_group(pa[:, 0:T], [(Wa[:, kc, jj * 128:(jj + 1) * 128], XN[:, kc, 0:T]) for kc in range(NCH)],
                                   r=[b_W[ia], bXN], w=[pab])
                        P.mm_group(pg[:, 0:T], [(Wg[:, kc, jj * 128:(jj + 1) * 128], XN[:, kc, 0:T]) for kc in range(NCH)],
                                   r=[b_W[ig], bXN], w=[pgb])
                        i2 = fc % 2
                        kb.op("act", lambda e: e.activation(out=SG[i2][:, 0:T], in_=pg[:, 0:T], func=AF.Silu),
                              r=[pgb], w=[b_SG[i2]])
                        kb.op("dve", lambda e: e.tensor_tensor(out=AT[:, fc, 0:T], in0=pa[:, 0:T], in1=SG[i2][:, 0:T],
                                                               op=ALU.mult), r=[pab, b_SG[i2]], w=[b_AT[fc]])
                if i + 1 < len(order):
                    prep(i + 1)
                for m in range(NCH):
                    iw = wcnt % NW
                    wcnt += 1
                    Wo = W[iw][:, 0:KF * 128].rearrange("p (k n) -> p k n", k=KF)
                    P.wget(f"fo{layer}_{half}_{m}", Wo, b_W[iw],
                           lambda: kb.dma("pool", Wo, wo[:, :, m * 128:(m + 1) * 128], w=[b_W[iw]]))
                    ir = rcnt % NR
                    rcnt += 1
                    kb.dma("act", XRg[ir][:, 0:T], HTv[:, m, t0:t0 + T], r=[tile_buf(t0)], w=[b_XRg[ir]])
                    po, pob = P.ps()
                    P.mm_group(po[:, 0:T], [(Wo[:, kc, :], AT[:, kc, 0:T]) for kc in range(KF)],
                               r=[b_W[iw], b_AT], w=[pob])
                    kb.op("dve", lambda e: e.scalar_tensor_tensor(
                        out=XRg[ir][:, 0:T], in0=po[:, 0:T], scalar=Gt[:, jn * 2 + s, m:m + 1], in1=XRg[ir][:, 0:T],
                        op0=ALU.mult, op1=ALU.add), r=[pob, b_G, b_XRg[ir]], w=[b_XRg[ir]])
                    kb.dma("act", HTv[:, m, t0:t0 + T], XRg[ir][:, 0:T], r=[b_XRg[ir]], w=[Buf("htst")])
        kb.barrier()

    def final_phase():
        with contextlib.ExitStack() as st:
            X = P.sb(st, "X", [128, NCH, 512]); bX = Buf("X")
            XN = P.sb(st, "XNf", [128, NCH, 512]); bXN = Buf("XNf")
            OT = [P.sb(st, "OT", [128, D]) for _ in range(2)]; b_OT = bufs(2, "OT")
            ntmp = alloc_norm_tmp(st, need_sq=True)
            oi = 0
            for (t0, T, s) in tiles:
                if s == 1:
                    continue
                load_x(X, bX, t0, T)
                norm_tile(ntmp, X, bX, XN, bXN, T, gfin, None)
                for tb in range(T // 128):
                    o = oi % 2
                    oi += 1
                    for q in range(4):
                        pt, pb = P.ps()
                        for c4 in range(4):
                            c = q * 4 + c4
                            P.transpose(pt[:, c4 * 128:(c4 + 1) * 128], XN[:, c, tb * 128:(tb + 1) * 128],
                                        r=[bXN, b_ident], w=[pb])
                        if q % 2:
                            kb.op("act", lambda e: e.copy(out=OT[o][:, q * 512:(q + 1) * 512], in_=pt[:]), r=[pb], w=[b_OT[o]])
                        else:
                            kb.op("dve", lambda e: e.tensor_copy(out=OT[o][:, q * 512:(q + 1) * 512], in_=pt[:]), r=[pb], w=[b_OT[o]])
                    r0 = t0 - CTX + tb * 128
                    kb.dma("sp", out[r0:r0 + 128, :], OT[o][:], r=[b_OT[o]], w=[Buf("outrow")])
        kb.barrier()


    def mtiles(Tm=256):
        tl = []
        for t0 in range(0, CTX, Tm):
            tl.append((t0, min(Tm, CTX - t0), 1))
        for t0 in range(0, SEQ, Tm):
            tl.append((CTX + t0, min(Tm, SEQ - t0), 0))
        return tl

    def mlstm_pass(idx, dirn, ctx_out):
        TT = 256
        wq = ml_w_in[idx].rearrange("(kc p) n -> p kc n", p=128)
        with contextlib.ExitStack() as st:
            X = P.sb(st, "X", [128, NCH, TT]); bX = Buf("X")
            XN = P.sb(st, "XN", [128, NCH, TT], BF16); bXN = Buf("XN")
            QT = P.sb(st, "QT", [128, NCH, TT], BF16); b_QT = Buf("QT")
            KT = P.sb(st, "KT", [128, NCH, TT], BF16); b_KT = Buf("KT")
            KK = P.sb(st, "KK", [128, 2, D], BF16); b_KK = Buf("KK")
            VH = P.sb(st, "VH", [128, 2, 8, 260], BF16); b_VH = Buf("VH")
            VC = P.sb(st, "VC", [128, 2, 8, 260], BF16); b_VC = Buf("VC")
            VR = P.sb(st, "VR", [128, 2, D], BF16); b_VR = Buf("VR")
            QTv = QTd.rearrange("(c p) t -> p c t", p=128)
            KTv = KTd.rearrange("(c p) t -> p c t", p=128)
            NW = 4
            W = [P.sb(st, "W", [128, 8192], BF16) for _ in range(NW)]; b_W = bufs(NW, "W")
            Wg = P.sb(st, "Wg", [128, NCH, 32], BF16); b_Wg = Buf("Wg")
            bg = P.sb(st, "bg", [128, 32]); b_bg = Buf("bg")
            C32 = P.sb(st, "C32", [128, 8, 2, 260]); b_C32 = [bufs(2, f"C32_{h}_") for h in range(8)]
            Cbf = P.sb(st, "Cbf", [128, 8, 2, 260], BF16); b_Cbf = [bufs(2, f"Cbf_{h}_") for h in range(8)]
            Z = P.sb(st, "Z", [128, 2, 32]); b_Z = Buf("Z")
            G = {}
            for nm in ("L1", "CS", "TOT", "U", "Wt", "Wh", "DEC", "T1"):
                G[nm] = (P.sb(st, nm, [128, 2, 8]), Buf(nm))
            HACC = P.sb(st, "HACC", [128, 8, 260]); b_HACC = bufs(8, "HACC")
            HO = [P.sb(st, "HO", [128, 8, 256]) for _ in range(2)]; b_HO = bufs(2, "HO")
            SM = [P.sb(st, "SM", [128, 4, 128], BF16) for _ in range(2)]; b_SM = bufs(2, "SM")
            sm = {}
            for nm in ("r", "nr", "f"):
                sm[nm] = (P.sb(st, nm, [128, 8]), Buf(nm))
            ntmp = alloc_norm_tmp(st, Tmax=TT)
            kb.dma("pool", Wg[:], wq[:, :, 4 * D:4 * D + 32], w=[b_Wg])
            kb.dma("sp", bg[:], ml_b_gate[idx:idx + 1, :].to_broadcast([128, 32]), w=[b_bg])
            kb.op("dve", lambda e: e.memset(C32[:], 0.0), w=b_C32)
            kb.op("dve", lambda e: e.memset(Cbf[:], 0.0), w=b_Cbf)
            tl = mtiles(TT)
            ctx_t = [t for t in tl if t[2] == 1]
            lat_t = [t for t in tl if t[2] == 0]
            order = (ctx_t + lat_t) if dirn == 0 else (ctx_t[::-1] + lat_t[::-1])
            wcnt = 0
            hoi = 0
            for (t0, T, s) in order:
                nb = T // 128
                need_out = (s == 0) or ctx_out
                Zdv = Zd[t0:t0 + T, :].rearrange("(t p) g -> p t g", p=128)
                KKv = KKd[t0:t0 + T, :].rearrange("(t p) f -> p t f", p=128)
                VRv = VRd[t0:t0 + T, :].rearrange("(t p) f -> p t f", p=128)
                if dirn == 0:
                    load_x(X, bX, t0, T)
                    norm_tile(ntmp, X, bX, XN, bXN, T, Asc[:, 2 + s, :], modT[:, 3, :, s])
                    pz, pzb = P.ps()
                    for tb in range(nb):
                        P.mm_group(pz[:, tb * 32:(tb + 1) * 32],
                                   [(XN[:, kc, tb * 128:(tb + 1) * 128], Wg[:, kc, :]) for kc in range(NCH)],
                                   r=[bXN, b_Wg], w=[pzb])
                    kb.op("dve", lambda e: e.tensor_tensor(out=Z[:, 0:nb, :], in0=pz[:, 0:nb * 32].rearrange("p (t g) -> p t g", g=32),
                                                           in1=bg[:].unsqueeze(1).to_broadcast([128, nb, 32]), op=ALU.add),
                          r=[pzb, b_bg], w=[b_Z])
                    kb.dma("sp", Zdv, Z[:, 0:nb, :], r=[b_Z], w=[Buf("zd")])
                else:
                    kb.dma("sp", Z[:, 0:nb, :], Zdv, w=[b_Z])
                    kb.dma("sp", QT[:, :, 0:T], QTv[:, :, t0:t0 + T], w=[b_QT])
                    kb.dma("sp", KT[:, :, 0:T], KTv[:, :, t0:t0 + T], w=[b_KT])
                    kb.dma("sp", KK[:, 0:nb, :], KKv, w=[b_KK])
                    kb.dma("sp", VR[:, 0:nb, :], VRv, w=[b_VR])
                ig = Z[:, 0:nb, dirn * 16:dirn * 16 + 8]
                fg = Z[:, 0:nb, dirn * 16 + 8:dirn * 16 + 16]
                L1, b_L1 = G["L1"]; CS, b_CS = G["CS"]; TOT, b_TOT = G["TOT"]; U, b_U = G["U"]
                Wt, b_Wt = G["Wt"]; Wh, b_Wh = G["Wh"]; DEC, b_DEC = G["DEC"]; T1, b_T1 = G["T1"]
                kb.op("act", lambda e: e.activation(out=L1[:, 0:nb, :], in_=fg, func=AF.Exp, scale=-1.0), r=[b_Z], w=[b_L1])
                kb.op("act", lambda e: e.activation(out=L1[:, 0:nb, :], in_=L1[:, 0:nb, :], func=AF.Ln, bias=1.0), r=[b_L1], w=[b_L1])
                pc, pcb = P.ps()
                for tb in range(nb):
                    P.mm_group(pc[:, tb * 8:(tb + 1) * 8], [(tri[:, dirn, :], L1[:, tb, :])], r=[b_tri, b_L1], w=[pcb])
                    P.mm_group(pc[:, 64 + tb * 8:64 + (tb + 1) * 8], [(tri[:, 2, :], L1[:, tb, :])], r=[b_tri, b_L1], w=[pcb])
                kb.op("dve", lambda e: e.tensor_copy(out=CS[:, 0:nb, :], in_=pc[:, 0:nb * 8].rearrange("p (t g) -> p t g", g=8)), r=[pcb], w=[b_CS])
                kb.op("dve", lambda e: e.tensor_copy(out=TOT[:, 0:nb, :], in_=pc[:, 64:64 + nb * 8].rearrange("p (t g) -> p t g", g=8)), r=[pcb], w=[b_TOT])
                kb.op("act", lambda e: e.activation(out=U[:, 0:nb, :], in_=CS[:, 0:nb, :], func=AF.Exp, scale=-1.0), r=[b_CS], w=[b_U])
                kb.op("act", lambda e: e.activation(out=DEC[:, 0:nb, :], in_=TOT[:, 0:nb, :], func=AF.Exp, scale=-1.0), r=[b_TOT], w=[b_DEC])
                kb.op("dve", lambda e: e.tensor_tensor(out=T1[:, 0:nb, :], in0=ig, in1=CS[:, 0:nb, :], op=ALU.add), r=[b_Z, b_CS], w=[b_T1])
                kb.op("act", lambda e: e.activation(out=Wt[:, 0:nb, :], in_=T1[:, 0:nb, :], func=AF.Exp), r=[b_T1], w=[b_Wt])
                kb.op("dve", lambda e: e.tensor_tensor(out=T1[:, 0:nb, :], in0=T1[:, 0:nb, :], in1=TOT[:, 0:nb, :], op=ALU.subtract), r=[b_T1, b_TOT], w=[b_T1])
                kb.op("act", lambda e: e.activation(out=Wh[:, 0:nb, :], in_=T1[:, 0:nb, :], func=AF.Exp), r=[b_T1], w=[b_Wh])
                if cfg.dbg < 2:
                    continue
                if dirn == 0:
                    for which in range(2):
                        dst, bdst = (QT, b_QT) if which == 0 else (KT, b_KT)
                        for cb in range(4):
                            iw = wcnt % NW; wcnt += 1
                            Wv = W[iw][:].rearrange("p (k n) -> p k n", k=NCH)
                            P.wget(f"ml{idx}_{which}_{cb}", Wv, b_W[iw],
                                   lambda: kb.dma("pool", Wv, wq[:, :, which * D + cb * 512:which * D + (cb + 1) * 512], w=[b_W[iw]]))
                            for fcn in range(4):
                                pp, ppb = P.ps()
                                P.mm_group(pp[:, 0:T], [(Wv[:, kc, fcn * 128:(fcn + 1) * 128], XN[:, kc, 0:T]) for kc in range(NCH)],
                                           r=[b_W[iw], bXN], w=[ppb])
                                sc = 0.0625 if which == 0 else 1.0
                                if fcn % 2 == 0:
                                    kb.op("act", lambda e: e.activation(out=dst[:, cb * 4 + fcn, 0:T], in_=pp[:, 0:T], func=AF.Copy, scale=sc),
                                          r=[ppb], w=[bdst])
                                else:
                                    kb.op("dve", lambda e: e.tensor_scalar(out=dst[:, cb * 4 + fcn, 0:T], in0=pp[:, 0:T], scalar1=sc, scalar2=None,
                                                                           op0=ALU.mult), r=[ppb], w=[bdst])
                    for which in range(2):
                        for cb in range(4):
                            iw = wcnt % NW; wcnt += 1
                            Wv = W[iw][:].rearrange("p (k n) -> p k n", k=NCH)
                            col0 = (1 + which) * D + cb * 512
                            P.wget(f"ml{idx}_{1 + which}_{cb}", Wv, b_W[iw],
                                   lambda: kb.dma("pool", Wv, wq[:, :, col0:col0 + 512], w=[b_W[iw]]))
                            for tb in range(nb):
                                pp, ppb = P.ps()
                                P.mm_group(pp[:, :], [(XN[:, kc, tb * 128:(tb + 1) * 128], Wv[:, kc, :]) for kc in range(NCH)],
                                           r=[b_W[iw], bXN], w=[ppb])
                                if which == 0:
                                    kb.op("act", lambda e: e.copy(out=KK[:, tb, cb * 512:(cb + 1) * 512], in_=pp[:, :]), r=[ppb], w=[b_KK])
                                else:
                                    kb.op("act", lambda e: e.copy(out=VR[:, tb, cb * 512:(cb + 1) * 512], in_=pp[:, :]), r=[ppb], w=[b_VR])
                                    for hh in range(2):
                                        h = cb * 2 + hh
                                        kb.op("dve", lambda e: e.tensor_scalar(out=VH[:, tb, h, 0:256], in0=pp[:, hh * 256:(hh + 1) * 256],
                                                                               scalar1=Wt[:, tb, h:h + 1], scalar2=None, op0=ALU.mult),
                                              r=[ppb, b_Wt], w=[b_VH])
                                        kb.op("act", lambda e: e.activation(out=VC[:, tb, h, 0:256], in_=pp[:, hh * 256:(hh + 1) * 256],
                                                                            func=AF.Copy, scale=Wh[:, tb, h:h + 1]),
                                              r=[ppb, b_Wh], w=[b_VC])
                    kb.dma("sp", QTv[:, :, t0:t0 + T], QT[:, :, 0:T], r=[b_QT], w=[Buf("qtd")])
                    kb.dma("sp", KTv[:, :, t0:t0 + T], KT[:, :, 0:T], r=[b_KT], w=[Buf("ktd")])
                    kb.dma("sp", KKv, KK[:, 0:nb, :], r=[b_KK], w=[Buf("kkd")])
                    kb.dma("sp", VRv, VR[:, 0:nb, :], r=[b_VR], w=[Buf("vrd")])
                else:
                    for tb in range(nb):
                        for h in range(8):
                            kb.op("dve", lambda e: e.tensor_scalar(out=VH[:, tb, h, 0:256], in0=VR[:, tb, h * 256:(h + 1) * 256],
                                                                   scalar1=Wt[:, tb, h:h + 1], scalar2=None, op0=ALU.mult),
                                  r=[b_VR, b_Wt], w=[b_VH])
                            kb.op("act", lambda e: e.activation(out=VC[:, tb, h, 0:256], in_=VR[:, tb, h * 256:(h + 1) * 256],
                                                                func=AF.Copy, scale=Wh[:, tb, h:h + 1]),
                                  r=[b_VR, b_Wh], w=[b_VC])
                kb.op("dve", lambda e: e.tensor_copy(out=VH[:, 0:nb, :, 256], in_=Wt[:, 0:nb, :]), r=[b_Wt], w=[b_VH])
                kb.op("dve", lambda e: e.tensor_copy(out=VC[:, 0:nb, :, 256], in_=Wh[:, 0:nb, :]), r=[b_Wh], w=[b_VC])
                if cfg.dbg < 3:
                    continue
                tbs = list(range(nb)) if dirn == 0 else list(range(nb))[::-1]
                for tb in tbs:
                    tsl = slice(tb * 128, (tb + 1) * 128)
                    for hg in range(2):
                        si = hg % 2
                        if need_out:
                            pS, pSb = P.ps()
                            for h4 in range(4):
                                h = hg * 4 + h4
                                P.mm_group(pS[:, h4 * 128:(h4 + 1) * 128],
                                           [(KT[:, h * 2 + dc, tsl], QT[:, h * 2 + dc, tsl]) for dc in range(2)],
                                           r=[b_KT, b_QT], w=[pSb])
                            kb.op("dve", lambda e: e.tensor_tensor(out=SM[si][:], in0=pS[:].rearrange("p (h l) -> p h l", h=4),
                                                                   in1=tri[:, dirn, :].unsqueeze(1).to_broadcast([128, 4, 128]), op=ALU.mult),
                                  r=[pSb, b_tri], w=[b_SM[si]])
                        for h4 in range(4):
                            h = hg * 4 + h4
                            if need_out:
                                pa, pab = P.ps()
                                P.mm_group(pa[:, 0:257],
                                           [(SM[si][:, h4, :], VH[:, tb, h, 0:257])] +
                                           [(QT[:, h * 2 + dc, tsl], Cbf[:, h, dc, 0:257]) for dc in range(2)],
                                           r=[b_SM[si], b_VH, b_QT, b_Cbf[h]], w=[pab])
                                kb.op("act", lambda e: e.copy(out=HACC[:, h, 0:257], in_=pa[:, 0:257]), r=[pab], w=[b_HACC[h]])
                            for dc in range(2):
                                pd, pdb = P.ps()
                                P.mm_group(pd[:, 0:257], [(KK[:, tb, h * 256 + dc * 128:h * 256 + (dc + 1) * 128], VC[:, tb, h, 0:257])],
                                           r=[b_KK, b_VC], w=[pdb])
                                kb.op("dve", lambda e: e.scalar_tensor_tensor(out=C32[:, h, dc, 0:257], in0=C32[:, h, dc, 0:257], scalar=DEC[:, tb, h:h + 1],
                                                                              in1=pd[:, 0:257], op0=ALU.mult, op1=ALU.add),
                                      r=[pdb, b_DEC, b_C32[h][dc]], w=[b_C32[h][dc]])
                                kb.op("pool", lambda e: e.tensor_copy(out=Cbf[:, h, dc, 0:257], in_=C32[:, h, dc, 0:257]),
                                      r=[b_C32[h][dc]], w=[b_Cbf[h][dc]])
                    if need_out:
                        r_, b_r = sm["r"]; nr_, b_nr = sm["nr"]; f_, b_f = sm["f"]
                        kb.op("dve", lambda e: e.tensor_tensor(out=r_[:], in0=HACC[:, :, 256], in1=U[:, tb, :], op=ALU.mult),
                              r=[b_HACC, b_U], w=[b_r])
                        kb.op("dve", lambda e: e.tensor_scalar(out=nr_[:], in0=r_[:], scalar1=-1.0, scalar2=None, op0=ALU.mult), r=[b_r], w=[b_nr])
                        kb.op("dve", lambda e: e.tensor_tensor(out=nr_[:], in0=nr_[:], in1=r_[:], op=ALU.max), r=[b_r, b_nr], w=[b_nr])
                        kb.op("dve", lambda e: e.tensor_scalar_max(out=nr_[:], in0=nr_[:], scalar1=1.0), r=[b_nr], w=[b_nr])
                        kb.op("dve", lambda e: e.reciprocal(out=nr_[:], in_=nr_[:]), r=[b_nr], w=[b_nr])
                        kb.op("dve", lambda e: e.tensor_tensor(out=f_[:], in0=nr_[:], in1=U[:, tb, :], op=ALU.mult), r=[b_nr, b_U], w=[b_f])
                        o = hoi % 2; hoi += 1
                        kb.op("dve", lambda e: e.tensor_tensor(out=HO[o][:], in0=HACC[:, :, 0:256],
                                                               in1=f_[:].unsqueeze(2).to_broadcast([128, 8, 256]), op=ALU.mult),
                              r=[b_HACC, b_f], w=[b_HO[o]])
                        kb.dma("sp", HD[dirn][t0 + tb * 128:t0 + (tb + 1) * 128, :], HO[o][:].rearrange("p h d -> p (h d)"),
                               r=[b_HO[o]], w=[Buf("hd")])
        kb.barrier()

    def mlstm_merge(idx, with_ctx):
        TT = 256
        wq = ml_w_in[idx].rearrange("(kc p) n -> p kc n", p=128)
        wo = ml_w_out[idx].rearrange("(kc p) n -> p kc n", p=128)
        with contextlib.ExitStack() as st:
            X = P.sb(st, "X", [128, NCH, TT]); bX = Buf("X")
            XN = P.sb(st, "XN", [128, NCH, TT], BF16); bXN = Buf("XN")
            HF = P.sb(st, "HF", [128, 2, D]); b_HF = Buf("HF")
            HB = P.sb(st, "HB", [128, 2, D]); b_HB = Buf("HB")
            SO = P.sb(st, "SO", [128, 2, D]); b_SO = Buf("SO")
            GTt = P.sb(st, "GTt", [128, NCH, TT], BF16); b_GTt = Buf("GTt")
            gh = P.sb(st, "gh", [128, D]); b_gh = Buf("gh")
            ssq = P.sb(st, "ssq", [128, 2, 8]); b_ssq = Buf("ssq")
            NW = 4
            W = [P.sb(st, "W", [128, 8192], BF16) for _ in range(NW)]; b_W = bufs(NW, "W")
            ntmp = alloc_norm_tmp(st, Tmax=TT)
            kb.dma("sp", gh[:], ml_g_head[idx:idx + 1, :].to_broadcast([128, D]), w=[b_gh])
            wcnt = 0
            for (t0, T, s) in mtiles(TT):
                if s == 1 and not with_ctx:
                    continue
                nb = T // 128
                load_x(X, bX, t0, T)
                norm_tile(ntmp, X, bX, XN, bXN, T, Asc[:, 2 + s, :], modT[:, 3, :, s])
                kb.dma("sp", HF[:, 0:nb, :], HD[0][t0:t0 + T, :].rearrange("(t p) f -> p t f", p=128), w=[b_HF])
                kb.dma("sp", HB[:, 0:nb, :], HD[1][t0:t0 + T, :].rearrange("(t p) f -> p t f", p=128), w=[b_HB])
                for cb in range(4):
                    iw = wcnt % NW; wcnt += 1
                    Wv = W[iw][:].rearrange("p (k n) -> p k n", k=NCH)
                    P.wget(f"ml{idx}_3_{cb}", Wv, b_W[iw],
                           lambda: kb.dma("pool", Wv, wq[:, :, 3 * D + cb * 512:3 * D + (cb + 1) * 512], w=[b_W[iw]]))
                    for tb in range(nb):
                        pp, ppb = P.ps()
                        P.mm_group(pp[:, :], [(XN[:, kc, tb * 128:(tb + 1) * 128], Wv[:, kc, :]) for kc in range(NCH)],
                                   r=[b_W[iw], bXN], w=[ppb])
                        kb.op("act", lambda e: e.activation(out=SO[:, tb, cb * 512:(cb + 1) * 512], in_=pp[:, :], func=AF.Sigmoid),
                              r=[ppb], w=[b_SO])
                kb.op("dve", lambda e: e.tensor_tensor(out=HF[:, 0:nb, :], in0=HF[:, 0:nb, :], in1=HB[:, 0:nb, :], op=ALU.add),
                      r=[b_HF, b_HB], w=[b_HF])
                kb.op("dve", lambda e: e.tensor_tensor(out=HB[:, 0:nb, :], in0=HF[:, 0:nb, :], in1=HF[:, 0:nb, :], op=ALU.mult),
                      r=[b_HF, b_HB], w=[b_HB])
                kb.op("dve", lambda e: e.tensor_reduce(out=ssq[:, 0:nb, :], in_=HB[:, 0:nb, :].rearrange("p t (h d) -> p t h d", h=8),
                                                       axis=AX.X, op=ALU.add), r=[b_HB], w=[b_ssq])
                kb.op("dve", lambda e: e.tensor_scalar(out=ssq[:, 0:nb, :], in0=ssq[:, 0:nb, :], scalar1=1.0 / 256, scalar2=EPS,
                                                       op0=ALU.mult, op1=ALU.add), r=[b_ssq], w=[b_ssq])
                kb.op("act", lambda e: e.activation(out=ssq[:, 0:nb, :], in_=ssq[:, 0:nb, :], func=AF.Sqrt), r=[b_ssq], w=[b_ssq])
                kb.op("dve", lambda e: e.reciprocal(out=ssq[:, 0:nb, :], in_=ssq[:, 0:nb, :]), r=[b_ssq], w=[b_ssq])
                for tb in range(nb):
                    kb.op("dve", lambda e: e.tensor_tensor(out=HF[:, tb, :].rearrange("p (h d) -> p h d", h=8),
                                                           in0=HF[:, tb, :].rearrange("p (h d) -> p h d", h=8),
                                                           in1=ssq[:, tb, :].unsqueeze(2).to_broadcast([128, 8, 256]), op=ALU.mult),
                          r=[b_HF, b_ssq], w=[b_HF])
                    kb.op("pool", lambda e: e.tensor_tensor(out=SO[:, tb, :], in0=SO[:, tb, :], in1=gh[:], op=ALU.mult),
                          r=[b_SO, b_gh], w=[b_SO])
                    kb.op("dve", lambda e: e.tensor_tensor(out=HF[:, tb, :], in0=HF[:, tb, :], in1=SO[:, tb, :], op=ALU.mult),
                          r=[b_HF, b_SO], w=[b_HF])
                    for q in range(4):
                        pt, pb = P.ps()
                        for c4 in range(4):
                            c = q * 4 + c4
                            P.transpose(pt[:, c4 * 128:(c4 + 1) * 128], HF[:, tb, c * 128:(c + 1) * 128], r=[b_HF, b_ident], w=[pb])
                        if q % 2:
                            kb.op("act", lambda e: e.copy(out=GTt[:, q * 4:(q + 1) * 4, tb * 128:(tb + 1) * 128],
                                                          in_=pt[:].rearrange("p (c t) -> p c t", c=4)), r=[pb], w=[b_GTt])
                        else:
                            kb.op("dve", lambda e: e.tensor_copy(out=GTt[:, q * 4:(q + 1) * 4, tb * 128:(tb + 1) * 128],
                                                                 in_=pt[:].rearrange("p (c t) -> p c t", c=4)), r=[pb], w=[b_GTt])
                for cb in range(4):
                    iw = wcnt % NW; wcnt += 1
                    Wv = W[iw][:].rearrange("p (k n) -> p k n", k=NCH)
                    P.wget(f"mlo{idx}_{cb}", Wv, b_W[iw],
                           lambda: kb.dma("pool", Wv, wo[:, :, cb * 512:(cb + 1) * 512], w=[b_W[iw]]))
                    for fcn in range(4):
                        m = cb * 4 + fcn
                        pp, ppb = P.ps()
                        P.mm_group(pp[:, 0:T], [(Wv[:, kc, fcn * 128:(fcn + 1) * 128], GTt[:, kc, 0:T]) for kc in range(NCH)],
                                   r=[b_W[iw], b_GTt], w=[ppb])
                        kb.op("dve", lambda e: e.scalar_tensor_tensor(out=X[:, m, 0:T], in0=pp[:, 0:T], scalar=Gt[:, 2 + s, m:m + 1],
                                                                      in1=X[:, m, 0:T], op0=ALU.mult, op1=ALU.add),
                              r=[ppb, b_G, bX], w=[bX])
                kb.dma("sp", HTv[:, :, t0:t0 + T], X[:, :, 0:T], r=[bX], w=[tile_buf(t0)])
        kb.barrier()

    def mlstm_layer(idx, has_next):
        mlstm_pass(idx, 0, has_next)
        if cfg.dbg >= 4:
            mlstm_pass(idx, 1, has_next)
        if cfg.dbg >= 5:
            mlstm_merge(idx, has_next)


    def wa_layer(idx, with_ctx):
        TT = 256
        wi = wa_w_in[idx].rearrange("(kc p) n -> p kc n", p=128)
        wo = wa_w_out[idx].rearrange("(kc p) n -> p kc n", p=128)
        NB = NT // 128
        CB = CTX // 128
        SCALE = 128 ** -0.5
        with contextlib.ExitStack() as st:
            X = P.sb(st, "X", [128, NCH, TT]); bX = Buf("X")
            XN = P.sb(st, "XN", [128, NCH, TT], BF16); bXN = Buf("XN")
            QT = P.sb(st, "QT", [128, NCH, TT], BF16); b_QT = Buf("QT")
            KTa = P.sb(st, "KTa", [128, 4, NT], BF16); b_KTa = bufs(NB, "KTa")
            Va = P.sb(st, "Va", [128, NB, 4, 132], BF16); b_Va = bufs(NB, "Va")
            NW = 4
            W = [P.sb(st, "W", [128, NCH, 256], BF16) for _ in range(NW)]; b_W = bufs(NW, "W")
            cosT = P.sb(st, "cosT", [128, TT]); sinT = P.sb(st, "sinT", [128, TT]); b_cs = Buf("cossin")
            rt = [P.sb(st, "rt", [128, TT]) for _ in range(2)]; b_rt = bufs(2, "rt")
            YACC = P.sb(st, "YACC", [128, 16, 132]); b_YACC = bufs(16, "YACC")
            Y = P.sb(st, "Y", [128, D]); b_Y = Buf("Y")
            PT = P.sb(st, "PT", [128, 5, 4, 128], BF16); b_PT = bufs(5, "PT")
            YT = P.sb(st, "YT", [128, NCH, TT], BF16); b_YT = Buf("YT")
            esink = P.sb(st, "esink", [128, 16]); b_es = Buf("esink")
            den = P.sb(st, "den", [128, 16]); b_den = Buf("den")
            ntmp = alloc_norm_tmp(st, Tmax=TT)
            kb.dma("sp", esink[:], wa_sink[idx:idx + 1, :].to_broadcast([128, 16]), w=[b_es])
            kb.op("act", lambda e: e.activation(out=esink[:], in_=esink[:], func=AF.Exp), r=[b_es], w=[b_es])
            kb.op("dve", lambda e: e.memset(Va[:, :, :, 128:129], 1.0), w=b_Va)
            wst = {"c": 0}

            def wload(col0, swapped):
                iw = wst["c"] % NW; wst["c"] += 1

                def loader():
                    if not swapped:
                        kb.dma("pool", W[iw][:], wi[:, :, col0:col0 + 256], w=[b_W[iw]])
                    else:
                        dv = W[iw][:].rearrange("p k (h two d) -> p k h two d", h=2, two=2)
                        sv = wi[:, :, col0:col0 + 256].rearrange("p k (h two d) -> p k h two d", h=2, two=2)
                        for hh in range(2):
                            kb.dma("pool", dv[:, :, hh, 0, :], sv[:, :, hh, 1, :], w=[b_W[iw]])
                            kb.dma("pool", dv[:, :, hh, 1, :], sv[:, :, hh, 0, :], w=[b_W[iw]])
                P.wget(f"wa{idx}_{col0}_{int(swapped)}", W[iw][:], b_W[iw], loader)
                return iw

            def proj_rope(col0, nheads, dst_fn, bdst, T, s, lt0, scale_q=1.0):
                for hb in range(nheads // 2):
                    iw = wload(col0 + hb * 256, False)
                    if s == 0:
                        iws = wload(col0 + hb * 256, True)
                    for hh in range(2):
                        h = hb * 2 + hh
                        pp, ppb = P.ps()
                        P.mm_group(pp[:, 0:T], [(W[iw][:, kc, hh * 128:(hh + 1) * 128], XN[:, kc, 0:T]) for kc in range(NCH)],
                                   r=[b_W[iw], bXN], w=[ppb])
                        if s == 1:
                            kb.op("act", lambda e: e.copy(out=dst_fn(h), in_=pp[:, 0:T]), r=[ppb], w=bdst)
                            continue
                        ps2, ps2b = P.ps()
                        P.mm_group(ps2[:, 0:T], [(W[iws][:, kc, hh * 128:(hh + 1) * 128], XN[:, kc, 0:T]) for kc in range(NCH)],
                                   r=[b_W[iws], bXN], w=[ps2b])
                        kb.op("dve", lambda e: e.tensor_tensor(out=rt[0][:, 0:T], in0=pp[:, 0:T], in1=cosT[:, 0:T], op=ALU.mult),
                              r=[ppb, b_cs], w=[b_rt[0]])
                        kb.op("dve", lambda e: e.tensor_tensor(out=rt[1][:, 0:T], in0=ps2[:, 0:T], in1=sinT[:, 0:T], op=ALU.mult),
                              r=[ps2b, b_cs], w=[b_rt[1]])
                        kb.op("dve", lambda e: e.tensor_tensor(out=dst_fn(h), in0=rt[0][:, 0:T], in1=rt[1][:, 0:T], op=ALU.add),
                              r=b_rt, w=bdst)

            for (t0, T, s) in mtiles(TT):
                nb = T // 128
                load_x(X, bX, t0, T)
                norm_tile(ntmp, X, bX, XN, bXN, T, Asc[:, 2 + s, :], modT[:, 3, :, s])
                if s == 0:
                    kb.dma("sp", cosT[:, 0:T], rope_cos[:, t0 - CTX:t0 - CTX + T], w=[b_cs])
                    kb.dma("sp", sinT[:, 0:T], rope_sin[:, t0 - CTX:t0 - CTX + T], w=[b_cs])
                blk0 = t0 // 128
                proj_rope(2048, 4, lambda h: KTa[:, h, t0:t0 + T], [b_KTa[blk0 + i] for i in range(nb)], T, s, t0)
                for vb in range(2):
                    iw = wload(2560 + vb * 256, False)
                    for tb in range(nb):
                        pp, ppb = P.ps()
                        P.mm_group(pp[:, 0:256], [(XN[:, kc, tb * 128:(tb + 1) * 128], W[iw][:, kc, :]) for kc in range(NCH)],
                                   r=[b_W[iw], bXN], w=[ppb])
                        kb.op("act", lambda e: e.copy(out=Va[:, blk0 + tb, vb * 2:vb * 2 + 2, 0:128],
                                                      in_=pp[:, 0:256].rearrange("p (h d) -> p h d", h=2)),
                              r=[ppb], w=[b_Va[blk0 + tb]])
            for (t0, T, s) in mtiles(TT):
                if s == 1 and not with_ctx:
                    continue
                nb = T // 128
                load_x(X, bX, t0, T)
                norm_tile(ntmp, X, bX, XN, bXN, T, Asc[:, 2 + s, :], modT[:, 3, :, s])
                if s == 0:
                    kb.dma("sp", cosT[:, 0:T], rope_cos[:, t0 - CTX:t0 - CTX + T], w=[b_cs])
                    kb.dma("sp", sinT[:, 0:T], rope_sin[:, t0 - CTX:t0 - CTX + T], w=[b_cs])
                proj_rope(0, 16, lambda h: QT[:, h, 0:T], [b_QT], T, s, t0)
                for tb in range(nb):
                    qblk = t0 // 128 + tb
                    qsl = slice(tb * 128, (tb + 1) * 128)
                    if s == 0:
                        kbl = []
                        if qblk - 1 >= CB:
                            kbl.append((qblk - 1, 1))
                        kbl.append((qblk, None))
                        if qblk + 1 < NB:
                            kbl.append((qblk + 1, 0))
                        kbl += [(c, None) for c in range(CB)]
                    else:
                        kbl = [(c, None) for c in range(CB)]
                    for kv in range(4):
                        for ki, (kblk, mk) in enumerate(kbl):
                            pS, pSb = P.ps()
                            P.mm_group(pS[:, :], [(KTa[:, kv, kblk * 128:(kblk + 1) * 128], QT[:, kv * 4:(kv + 1) * 4, qsl])],
                                       r=[b_KTa[kblk], b_QT], w=[pSb])
                            kb.op("act", lambda e: e.activation(out=PT[:, ki, :, :], in_=pS[:, :].rearrange("p (g q) -> p g q", g=4),
                                                                func=AF.Exp, scale=SCALE), r=[pSb], w=[b_PT[ki]])
                            if mk is not None:
                                kb.op("dve", lambda e: e.tensor_tensor(out=PT[:, ki, :, :], in0=PT[:, ki, :, :],
                                                                       in1=tri[:, mk, :].unsqueeze(1).to_broadcast([128, 4, 128]), op=ALU.mult),
                                      r=[b_PT[ki], b_tri], w=[b_PT[ki]])
                        for g2 in range(2):
                            pa, pab = P.ps()
                            for gg in range(2):
                                g = g2 * 2 + gg
                                P.mm_group(pa[:, gg * 132:gg * 132 + 129],
                                           [(PT[:, ki, g, :], Va[:, kblk, kv, 0:129]) for ki, (kblk, mk) in enumerate(kbl)],
                                           r=[b_PT[0:len(kbl)], [b_Va[kblk] for (kblk, mk) in kbl]], w=[pab])
                            h0 = kv * 4 + g2 * 2
                            kb.op("act", lambda e: e.copy(out=YACC[:, h0:h0 + 2, :], in_=pa[:, 0:264].rearrange("p (g d) -> p g d", g=2)),
                                  r=[pab], w=[b_YACC[h0], b_YACC[h0 + 1]])
                    kb.op("dve", lambda e: e.tensor_tensor(out=den[:], in0=YACC[:, :, 128], in1=esink[:], op=ALU.add),
                          r=[b_YACC, b_es], w=[b_den])
                    kb.op("dve", lambda e: e.reciprocal(out=den[:], in_=den[:]), r=[b_den], w=[b_den])
                    kb.op("dve", lambda e: e.tensor_tensor(out=Y[:].rearrange("p (h d) -> p h d", h=16), in0=YACC[:, :, 0:128],
                                                           in1=den[:].unsqueeze(2).to_broadcast([128, 16, 128]), op=ALU.mult),
                          r=[b_YACC, b_den], w=[b_Y])
                    for q in range(4):
                        pt, pb = P.ps()
                        for c4 in range(4):
                            c = q * 4 + c4
                            P.transpose(pt[:, c4 * 128:(c4 + 1) * 128], Y[:, c * 128:(c + 1) * 128], r=[b_Y, b_ident], w=[pb])
                        if q % 2:
                            kb.op("act", lambda e: e.copy(out=YT[:, q * 4:(q + 1) * 4, qsl], in_=pt[:].rearrange("p (c t) -> p c t", c=4)),
                                  r=[pb], w=[b_YT])
                        else:
                            kb.op("dve", lambda e: e.tensor_copy(out=YT[:, q * 4:(q + 1) * 4, qsl], in_=pt[:].rearrange("p (c t) -> p c t", c=4)),
                                  r=[pb], w=[b_YT])
                for cb in range(8):
                    iw = wst["c"] % NW; wst["c"] += 1
                    P.wget(f"wao{idx}_{cb}", W[iw][:], b_W[iw],
                           lambda: kb.dma("pool", W[iw][:], wo[:, :, cb * 256:(cb + 1) * 256], w=[b_W[iw]]))
                    for fcn in range(2):
                        m = cb * 2 + fcn
                        pp, ppb = P.ps()
                        P.mm_group(pp[:, 0:T], [(W[iw][:, kc, fcn * 128:(fcn + 1) * 128], YT[:, kc, 0:T]) for kc in range(NCH)],
                                   r=[b_W[iw], b_YT], w=[ppb])
                        kb.op("dve", lambda e: e.scalar_tensor_tensor(out=X[:, m, 0:T], in0=pp[:, 0:T], scalar=Gt[:, 2 + s, m:m + 1],
                                                                      in1=X[:, m, 0:T], op0=ALU.mult, op1=ALU.add),
                              r=[ppb, b_G, bX], w=[bX])
                kb.dma("sp", HTv[:, :, t0:t0 + T], X[:, :, 0:T], r=[bX], w=[tile_buf(t0)])
        kb.barrier()


    TWO_PI = float(2.0 * math.pi)

    def s5_layer(idx, with_ctx):
        I32 = mybir.dt.int32
        blocks = [(t0, T) for (t0, T, s) in tiles]
        with contextlib.ExitStack() as lst:
            MAG = P.sb(lst, "MAG", [128, 2, 64]); THN = P.sb(lst, "THN", [128, 2, 64]); b_prm = Buf("s5prm")
            BB = P.sb(lst, "BB", [128, 2, 2, 64, 16]); b_BB = Buf("BB")
            DSK = P.sb(lst, "DSK", [128, NCH]); b_DSK = Buf("DSK")
            iota1 = P.sb(lst, "iota1", [128, 512]); b_iota = Buf("iota")
            kb.dma("sp", iota1[:], iota_in[:, :], w=[b_iota])

            def sincos(st, src, n, sin_out, cos_out, b_src, b_out):
                xi = P.sb(st, "xi", [128, n], I32); xf = P.sb(st, "xf", [128, n]); xr = P.sb(st, "xr", [128, n])
                b1, b2, b3 = Buf("xi"), Buf("xf"), Buf("xr")
                for (off, dst) in ((0.0, sin_out), (0.25, cos_out)):
                    if off == 0.0:
                        kb.op("dve", lambda e: e.tensor_copy(out=xr[:], in_=src), r=[b_src], w=[b3])
                    else:
                        kb.op("dve", lambda e: e.tensor_scalar(out=xr[:], in0=src, scalar1=off, scalar2=None, op0=ALU.add), r=[b_src], w=[b3])
                    kb.op("dve", lambda e: e.tensor_copy(out=xi[:], in_=xr[:]), r=[b3], w=[b1])
                    kb.op("dve", lambda e: e.tensor_copy(out=xf[:], in_=xi[:]), r=[b1], w=[b2])
                    kb.op("dve", lambda e: e.tensor_tensor(out=xr[:], in0=xr[:], in1=xf[:], op=ALU.subtract), r=[b3, b2], w=[b3])
                    kb.op("act", lambda e: e.activation(out=dst, in_=xr[:], func=AF.Sin, scale=TWO_PI), r=[b3], w=[b_out])

            with contextlib.ExitStack() as st:
                rows = P.sb(st, "rows", [64, 3, 128]); b_rows = Buf("rows")
                ldt = P.sb(st, "ldt", [64, 2]); b_ldt = Buf("ldt")
                PR = P.sb(st, "PR", [128, 3, 64]); b_PR = Buf("PR")
                RB = P.sb(st, "RB", [128, 2, 64, 16]); b_RB = Buf("RB")
                tt = {}
                for nm in ("dt", "are", "th", "sn", "cs", "lbr", "lbi", "nr", "den", "fr", "fi", "t1", "t2"):
                    tt[nm] = P.sb(st, nm, [128, 64])
                b_t = Buf("s5tmp")
                dsr = P.sb(st, "dsr", [NCH, 128]); b_dsr = Buf("dsr")
                kb.dma("sp", dsr[:], s5_d_skip[idx, :, :], w=[b_dsr])
                pt, pb = P.ps()
                P.transpose(pt[:, 0:NCH], dsr[:, :], r=[b_dsr, b_ident], w=[pb])
                kb.op("dve", lambda e: e.tensor_copy(out=DSK[:], in_=pt[:, 0:NCH]), r=[pb], w=[b_DSK])
                for d in range(2):
                    kb.dma("sp", rows[:, 0, :], s5_lam_re[idx, d].rearrange("(P gl) p -> P (gl p)", gl=2), w=[b_rows])
                    kb.dma("sp", rows[:, 1, :], s5_lam_im[idx, d].rearrange("(P gl) p -> P (gl p)", gl=2), w=[b_rows])
                    kb.dma("sp", ldt[:], s5_log_dt[idx, d].rearrange("(P gl) -> P gl", gl=2), w=[b_ldt])
                    kb.op("dve", lambda e: e.tensor_copy(out=rows[:, 2, :].rearrange("P (gl p) -> P gl p", gl=2),
                                                         in_=ldt[:].unsqueeze(2).to_broadcast([64, 2, 64])), r=[b_ldt], w=[b_rows])
                    pt, pb = P.ps()
                    for k in range(3):
                        P.transpose(pt[:, k * 64:(k + 1) * 64], rows[:, k, :], r=[b_rows, b_ident], w=[pb])
                    kb.op("dve", lambda e: e.tensor_copy(out=PR[:], in_=pt[:, 0:192].rearrange("p (k n) -> p k n", k=3)), r=[pb], w=[b_PR])
                    LRE, LIM, LDT = PR[:, 0, :], PR[:, 1, :], PR[:, 2, :]
                    kb.op("act", lambda e: e.activation(out=tt["dt"][:], in_=LDT, func=AF.Exp), r=[b_PR], w=[b_t])
                    kb.op("dve", lambda e: e.tensor_tensor(out=tt["are"][:], in0=LRE, in1=tt["dt"][:], op=ALU.mult), r=[b_PR, b_t], w=[b_t])
                    kb.op("act", lambda e: e.activation(out=MAG[:, d, :], in_=tt["are"][:], func=AF.Exp), r=[b_t], w=[b_prm])
                    kb.op("dve", lambda e: e.tensor_tensor(out=tt["th"][:], in0=LIM, in1=tt["dt"][:], op=ALU.mult), r=[b_PR, b_t], w=[b_t])
                    kb.op("dve", lambda e: e.tensor_scalar(out=THN[:, d, :], in0=tt["th"][:], scalar1=1.0 / TWO_PI, scalar2=None, op0=ALU.mult),
                          r=[b_t], w=[b_prm])
                    with contextlib.ExitStack() as st2:
                        sincos(st2, THN[:, d, :], 64, tt["sn"][:], tt["cs"][:], b_prm, b_t)
                    kb.op("dve", lambda e: e.tensor_tensor(out=tt["lbr"][:], in0=MAG[:, d, :], in1=tt["cs"][:], op=ALU.mult), r=[b_prm, b_t], w=[b_t])
                    kb.op("dve", lambda e: e.tensor_tensor(out=tt["lbi"][:], in0=MAG[:, d, :], in1=tt["sn"][:], op=ALU.mult), r=[b_prm, b_t], w=[b_t])
                    kb.op("dve", lambda e: e.tensor_scalar(out=tt["nr"][:], in0=tt["lbr"][:], scalar1=-1.0, scalar2=None, op0=ALU.add), r=[b_t], w=[b_t])
                    kb.op("dve", lambda e: e.tensor_tensor(out=tt["t1"][:], in0=LRE, in1=LRE, op=ALU.mult), r=[b_PR], w=[b_t])
                    kb.op("dve", lambda e: e.tensor_tensor(out=tt["t2"][:], in0=LIM, in1=LIM, op=ALU.mult), r=[b_PR, b_t], w=[b_t])
                    kb.op("dve", lambda e: e.tensor_tensor(out=tt["den"][:], in0=tt["t1"][:], in1=tt["t2"][:], op=ALU.add), r=[b_t], w=[b_t])
                    kb.op("dve", lambda e: e.reciprocal(out=tt["den"][:], in_=tt["den"][:]), r=[b_t], w=[b_t])
                    kb.op("dve", lambda e: e.tensor_tensor(out=tt["t1"][:], in0=tt["nr"][:], in1=LRE, op=ALU.mult), r=[b_PR, b_t], w=[b_t])
                    kb.op("dve", lambda e: e.tensor_tensor(out=tt["t2"][:], in0=tt["lbi"][:], in1=LIM, op=ALU.mult), r=[b_PR, b_t], w=[b_t])
                    kb.op("dve", lambda e: e.tensor_tensor(out=tt["fr"][:], in0=tt["t1"][:], in1=tt["t2"][:], op=ALU.add), r=[b_t], w=[b_t])
                    kb.op("dve", lambda e: e.tensor_tensor(out=tt["fr"][:], in0=tt["fr"][:], in1=tt["den"][:], op=ALU.mult), r=[b_t], w=[b_t])
                    kb.op("dve", lambda e: e.tensor_tensor(out=tt["t1"][:], in0=tt["lbi"][:], in1=LRE, op=ALU.mult), r=[b_PR, b_t], w=[b_t])
                    kb.op("dve", lambda e: e.tensor_tensor(out=tt["t2"][:], in0=tt["nr"][:], in1=LIM, op=ALU.mult), r=[b_PR, b_t], w=[b_t])
                    kb.op("dve", lambda e: e.tensor_tensor(out=tt["fi"][:], in0=tt["t1"][:], in1=tt["t2"][:], op=ALU.subtract), r=[b_t], w=[b_t])
                    kb.op("dve", lambda e: e.tensor_tensor(out=tt["fi"][:], in0=tt["fi"][:], in1=tt["den"][:], op=ALU.mult), r=[b_t], w=[b_t])
                    kb.dma("sp", RB[:, 0, :, :], s5_b_re[idx, d].rearrange("(P gl) p c -> (gl p) P c", gl=2), w=[b_RB])
                    kb.dma("sp", RB[:, 1, :, :], s5_b_im[idx, d].rearrange("(P gl) p c -> (gl p) P c", gl=2), w=[b_RB])
                    frb = tt["fr"][:].unsqueeze(2).to_broadcast([128, 64, 16])
                    fib = tt["fi"][:].unsqueeze(2).to_broadcast([128, 64, 16])
                    T3 = P.sb(st, "T3", [128, 64, 16]); b_T3 = Buf("T3")
                    kb.op("dve", lambda e: e.tensor_tensor(out=BB[:, d, 0], in0=RB[:, 0], in1=frb, op=ALU.mult), r=[b_RB, b_t], w=[b_BB])
                    kb.op("dve", lambda e: e.tensor_tensor(out=T3[:], in0=RB[:, 1], in1=fib, op=ALU.mult), r=[b_RB, b_t], w=[b_T3])
                    kb.op("dve", lambda e: e.tensor_tensor(out=BB[:, d, 0], in0=BB[:, d, 0], in1=T3[:], op=ALU.subtract), r=[b_T3, b_BB], w=[b_BB])
                    kb.op("dve", lambda e: e.tensor_tensor(out=BB[:, d, 1], in0=RB[:, 1], in1=frb, op=ALU.mult), r=[b_RB, b_t], w=[b_BB])
                    kb.op("dve", lambda e: e.tensor_tensor(out=T3[:], in0=RB[:, 0], in1=fib, op=ALU.mult), r=[b_RB, b_t, b_BB], w=[b_T3])
                    kb.op("dve", lambda e: e.tensor_tensor(out=BB[:, d, 1], in0=BB[:, d, 1], in1=T3[:], op=ALU.add), r=[b_T3, b_BB], w=[b_BB])
            kb.barrier()

            with contextlib.ExitStack() as st:
                X = P.sb(st, "X", [128, NCH, 512]); bX = Buf("X")
                XN = P.sb(st, "XN", [128, NCH, 512], BF16); bXN = Buf("XN")
                UO = P.sb(st, "UO", [128, NCH, 512]); b_UO = Buf("UO")
                NW = 3
                W = [P.sb(st, "W", [128, NCH, 512], BF16) for _ in range(NW)]; b_W = bufs(NW, "W")
                ntmp = alloc_norm_tmp(st)
                wi = s5_w_in[idx].rearrange("(kc p) n -> p kc n", p=128)
                UTv = UTd.rearrange("(c p) t -> p c t", p=128)
                wc = 0
                for (t0, T, s) in tiles:
                    load_x(X, bX, t0, T)
                    norm_tile(ntmp, X, bX, XN, bXN, T, Asc[:, 2 + s, :], modT[:, 3, :, s])
                    for cb in range(4):
                        iw = wc % NW; wc += 1
                        P.wget(f"s5i{idx}_{cb}", W[iw][:], b_W[iw],
                               lambda: kb.dma("pool", W[iw][:], wi[:, :, cb * 512:(cb + 1) * 512], w=[b_W[iw]]))
                        for fcn in range(4):
                            pp, ppb = P.ps()
                            P.mm_group(pp[:, 0:T], [(W[iw][:, kc, fcn * 128:(fcn + 1) * 128], XN[:, kc, 0:T]) for kc in range(NCH)],
                                       r=[b_W[iw], bXN], w=[ppb])
                            if fcn % 2:
                                kb.op("act", lambda e: e.copy(out=UO[:, cb * 4 + fcn, 0:T], in_=pp[:, 0:T]), r=[ppb], w=[b_UO])
                            else:
                                kb.op("dve", lambda e: e.tensor_copy(out=UO[:, cb * 4 + fcn, 0:T], in_=pp[:, 0:T]), r=[ppb], w=[b_UO])
                    kb.dma("sp", UTv[:, :, t0:t0 + T], UO[:, :, 0:T], r=[b_UO], w=[Buf("utd")])
            kb.barrier()

            with contextlib.ExitStack() as st:
                UT = P.sb(st, "UT", [128, NT]); b_UT = Buf("UT")
                YT = P.sb(st, "YT", [128, NT]); b_YT = Buf("YT")
                ZT = P.sb(st, "ZT", [128, NT], BF16); b_ZT = Buf("ZT")
                CRW = P.sb(st, "CRW", [128, 2, 2, 64]); b_CRW = Buf("CRW")
                CT = P.sb(st, "CT", [128, 2, 128]); b_CT = Buf("CT")
                LB = P.sb(st, "LB", [128, 4, 2, 128]); b_LB = Buf("LB")
                LC = P.sb(st, "LC", [128, 4, 2, 128]); b_LC = Buf("LC")
                SRC = P.sb(st, "SRC", [128, 128]); b_SRC = Buf("SRC")
                TAB = P.sb(st, "TAB", [128, 4, 2, 512]); b_TAB = bufs(4, "TAB")
                ANG = P.sb(st, "ANG", [128, 512]); b_ANG = Buf("ANG")
                STt = P.sb(st, "STt", [128, 4, 2]); b_ST = bufs(4, "ST")
                WORK = P.sb(st, "WORK", [128, 24 * 512])
                WK = {}
                for ni, nm in enumerate(("VR", "VI", "a", "b", "a2", "b2")):
                    WK[nm] = ([WORK[:, (ni * 4 + j) * 512:(ni * 4 + j + 1) * 512] for j in range(4)], bufs(4, nm))
                PQ = P.sb(st, "PQ", [128, 4, 4, 512], BF16); b_PQ = bufs(4, "PQ")
                UTb = P.sb(st, "UTb", [128, NT], BF16); b_UTb = Buf("UTb")
                LBb = P.sb(st, "LBb", [128, 4, 2, 128], BF16); b_LBb = Buf("LBb")
                LCb = P.sb(st, "LCb", [128, 4, 3, 128], BF16); b_LCb = Buf("LCb")
                stmp = P.sb(st, "stmp", [128, 4, 4]); b_stmp = bufs(4, "stmp")
                nident = P.sb(st, "nident", [128, 128]); b_nident = Buf("nident")
                kb.op("dve", lambda e: e.tensor_scalar(out=nident[:], in0=ident[:], scalar1=-1.0, scalar2=None, op0=ALU.mult), r=[b_ident], w=[b_nident])
                all_wk = [WK[nm][1] for nm in WK]
                G1 = WORK[:, 0:NT]; b_G1 = Buf("G1")
                sc_st = contextlib.ExitStack(); st.enter_context(sc_st)
                xi_ = P.sb(st, "xi", [128, 512], I32); xf_ = P.sb(st, "xf", [128, 512]); xr_ = P.sb(st, "xr", [128, 512])
                b_x1, b_x2, b_x3 = Buf("xi"), Buf("xf"), Buf("xr")

                def sincos512(src, b_src, sin_out, cos_out, b_out):
                    for (off, dst) in ((0.0, sin_out), (0.25, cos_out)):
                        kb.op("dve", lambda e: e.tensor_scalar(out=xr_[:], in0=src, scalar1=off, scalar2=None, op0=ALU.add), r=[b_src], w=[b_x3])
                        kb.op("dve", lambda e: e.tensor_copy(out=xi_[:], in_=xr_[:]), r=[b_x3], w=[b_x1])
                        kb.op("dve", lambda e: e.tensor_copy(out=xf_[:], in_=xi_[:]), r=[b_x1], w=[b_x2])
                        kb.op("dve", lambda e: e.tensor_tensor(out=xr_[:], in0=xr_[:], in1=xf_[:], op=ALU.subtract), r=[b_x3, b_x2], w=[b_x3])
                        kb.op("act", lambda e: e.activation(out=dst, in_=xr_[:], func=AF.Sin, scale=TWO_PI), r=[b_x3], w=[b_out])

                ui = 0
                for c in range(NCH):
                    kb.dma("sp", UT[:], UTd[c * 128:(c + 1) * 128, :], w=[b_UT])
                    kb.op("dve", lambda e: e.tensor_scalar(out=YT[:], in0=UT[:], scalar1=DSK[:, c:c + 1], scalar2=None, op0=ALU.mult),
                          r=[b_UT, b_DSK], w=[b_YT])
                    kb.op("act", lambda e: e.copy(out=UTb[:], in_=UT[:]), r=[b_UT], w=[b_UTb])
                    for d in range(2):
                        for ri, src in enumerate((s5_c_re, s5_c_im)):
                            cv = src[idx, d, c * 8:(c + 1) * 8].rearrange("g c p -> (g c) p")
                            kb.dma("sp", CRW[:, ri, 0, :], cv, w=[b_CRW])
                            kb.dma("sp", CRW[:, ri, 1, :], cv, w=[b_CRW])
                        pt, pb = P.ps()
                        for ri in range(2):
                            P.transpose(pt[:, ri * 128:(ri + 1) * 128], CRW[:, ri, :, :].rearrange("k a p -> k (a p)"), r=[b_CRW, b_ident], w=[pb])
                        kb.op("dve", lambda e: e.tensor_copy(out=CT[:], in_=pt[:, 0:256].rearrange("p (r k) -> p r k", r=2)), r=[pb], w=[b_CT])
                        kb.op("dve", lambda e: e.memset(LC[:], 0.0), w=[b_LC])
                        for j in range(4):
                            for ri in range(2):
                                sgn = 1.0 if ri == 0 else -1.0
                                for gl in range(2):
                                    ps_, ks_ = slice(gl * 64, (gl + 1) * 64), slice(32 * j + 16 * gl, 32 * j + 16 * gl + 16)
                                    kb.op("dve", lambda e: e.tensor_scalar(out=LC[ps_, j, ri, ks_], in0=CT[ps_, ri, ks_], scalar1=sgn, scalar2=None,
                                                                           op0=ALU.mult), r=[b_CT], w=[b_LC])
                        for j in range(4):
                            Pp = 4 * c + j
                            for ri in range(2):
                                kb.op("dve", lambda e: e.memset(SRC[:], 0.0), w=[b_SRC])
                                for gl in range(2):
                                    ps_, ks_ = slice(gl * 64, (gl + 1) * 64), slice(32 * j + 16 * gl, 32 * j + 16 * gl + 16)
                                    kb.op("dve", lambda e: e.tensor_copy(out=SRC[ps_, ks_], in_=BB[ps_, d, ri, Pp, :]), r=[b_BB], w=[b_SRC])
                                pt, pb = P.ps()
                                P.transpose(pt[:, 0:128], SRC[:, :], r=[b_SRC, b_ident], w=[pb])
                                kb.op("dve", lambda e: e.tensor_copy(out=LB[:, j, ri, :], in_=pt[:, 0:128]), r=[pb], w=[b_LB])
                            kb.op("dve", lambda e: e.tensor_scalar(out=ANG[:], in0=iota1[:], scalar1=THN[:, d, Pp:Pp + 1], scalar2=None, op0=ALU.mult),
                                  r=[b_iota, b_prm], w=[b_ANG])
                            sincos512(ANG[:], b_ANG, TAB[:, j, 0, :], TAB[:, j, 1, :], b_TAB[j])
                        kb.op("dve", lambda e: e.tensor_copy(out=LBb[:], in_=LB[:]), r=[b_LB], w=[b_LBb])
                        kb.op("dve", lambda e: e.tensor_copy(out=LCb[:, :, 0:2, :], in_=LC[:]), r=[b_LC], w=[b_LCb])
                        kb.op("dve", lambda e: e.tensor_scalar(out=LCb[:, :, 2, :], in0=LC[:, :, 0, :], scalar1=-1.0, scalar2=None, op0=ALU.mult),
                              r=[b_LC], w=[b_LCb])
                        kb.op("dve", lambda e: e.memset(STt[:], 0.0), w=b_ST)
                        ctxb = [b for b in blocks if b[0] < CTX]
                        latb = [b for b in blocks if b[0] >= CTX]
                        order = (ctxb + latb) if d == 0 else (ctxb[::-1] + latb[::-1])
                        for (t0, T) in order:
                            need_y = (t0 >= CTX) or with_ctx
                            R4 = range(4)
                            sn = [TAB[:, j, 0, 0:T] for j in R4]
                            cs = [TAB[:, j, 1, 0:T] for j in R4]
                            Wt_ = lambda nm, j: WK[nm][0][j][:, 0:T]
                            Wb_ = lambda nm, j: WK[nm][1][j]
                            for j in R4:
                                pr, prb = P.ps()
                                pi_, pib = P.ps()
                                P.mm_group(pr[:, 0:T], [(LBb[:, j, 0, :], UTb[:, t0:t0 + T])], r=[b_LBb, b_UTb], w=[prb])
                                P.mm_group(pi_[:, 0:T], [(LBb[:, j, 1, :], UTb[:, t0:t0 + T])], r=[b_LBb, b_UTb], w=[pib])
                                vr_in = pr[:, 0:T] if d == 0 else pr[:, 0:T][:, ::-1]
                                vi_in = pi_[:, 0:T] if d == 0 else pi_[:, 0:T][:, ::-1]
                                kb.op("act", lambda e: e.copy(out=Wt_("VR", j), in_=vr_in), r=[prb], w=[Wb_("VR", j)])
                                kb.op("act", lambda e: e.copy(out=Wt_("VI", j), in_=vi_in), r=[pib], w=[Wb_("VI", j)])
                            for j in R4:
                                for (o_, i0, tb_) in (("a", "VR", cs), ("b", "VI", sn), ("a2", "VI", cs), ("b2", "VR", sn)):
                                    kb.op("dve", lambda e: e.tensor_tensor(out=Wt_(o_, j), in0=Wt_(i0, j), in1=tb_[j], op=ALU.mult),
                                          r=[Wb_(i0, j), b_TAB[j]], w=[Wb_(o_, j)])
                            pv = {}
                            for j in R4:
                                pvr, pvrb = P.ps()
                                pvi, pvib = P.ps()
                                P.mm_group(pvr[:, 0:T], [(ident[:, :], Wt_("a", j)), (ident[:, :], Wt_("b", j))],
                                           r=[b_ident, Wb_("a", j), Wb_("b", j)], w=[pvrb])
                                P.mm_group(pvi[:, 0:T], [(ident[:, :], Wt_("a2", j)), (nident[:, :], Wt_("b2", j))],
                                           r=[b_ident, b_nident, Wb_("a2", j), Wb_("b2", j)], w=[pvib])
                                pv[j] = (pvr, pvrb, pvi, pvib)
                            for j in R4:
                                Pp = 4 * c + j
                                pvr, pvrb, pvi, pvib = pv[j]
                                magb = MAG[:, d, Pp:Pp + 1].to_broadcast([128, T])
                                kb.op("dve", lambda e: e.tensor_tensor_scan(out=Wt_("VR", j), data0=magb, data1=pvr[:, 0:T], initial=STt[:, j, 0:1],
                                                                            op0=ALU.mult, op1=ALU.add), r=[pvrb, b_prm, b_ST[j]], w=[Wb_("VR", j)])
                                kb.op("dve", lambda e: e.tensor_tensor_scan(out=Wt_("VI", j), data0=magb, data1=pvi[:, 0:T], initial=STt[:, j, 1:2],
                                                                            op0=ALU.mult, op1=ALU.add), r=[pvib, b_prm, b_ST[j]], w=[Wb_("VI", j)])
                            for j in R4:
                                for (q_, i0, tb_) in ((0, "VR", cs), (1, "VI", sn), (2, "VI", cs), (3, "VR", sn)):
                                    kb.op("dve", lambda e: e.tensor_tensor(out=PQ[:, j, q_, 0:T], in0=Wt_(i0, j), in1=tb_[j], op=ALU.mult),
                                          r=[Wb_(i0, j), b_TAB[j]], w=[b_PQ[j]])
                                zrl, zil = Wt_("VR", j)[:, T - 1:T], Wt_("VI", j)[:, T - 1:T]
                                csl, snl = TAB[:, j, 1, T - 1:T], TAB[:, j, 0, T - 1:T]
                                for (k_, i0_, tb_) in ((0, zil, snl), (1, zrl, csl), (2, zil, csl), (3, zrl, snl)):
                                    kb.op("pool", lambda e: e.tensor_tensor(out=stmp[:, j, k_:k_ + 1], in0=i0_, in1=tb_, op=ALU.mult),
                                          r=[Wb_("VI", j), Wb_("VR", j), b_TAB[j]], w=[b_stmp[j]])
                                kb.op("pool", lambda e: e.tensor_tensor(out=STt[:, j, 0:1], in0=stmp[:, j, 1:2], in1=stmp[:, j, 0:1], op=ALU.subtract),
                                      r=[b_stmp[j]], w=[b_ST[j]])
                                kb.op("pool", lambda e: e.tensor_tensor(out=STt[:, j, 1:2], in0=stmp[:, j, 2:3], in1=stmp[:, j, 3:4], op=ALU.add),
                                      r=[b_stmp[j]], w=[b_ST[j]])
                            if need_y:
                                py, pyb = P.ps()
                                prs = []
                                for j in range(4):
                                    for (q_, li) in ((0, 0), (1, 2), (2, 1), (3, 1)):
                                        xv = PQ[:, j, q_, 0:T]
                                        if d == 1:
                                            xv = xv[:, ::-1]
                                        prs.append((LCb[:, j, li, :], xv))
                                P.mm_group(py[:, 0:T], prs, r=[b_LCb, b_PQ], w=[pyb])
                                kb.op("dve", lambda e: e.tensor_tensor(out=YT[:, t0:t0 + T], in0=YT[:, t0:t0 + T], in1=py[:, 0:T], op=ALU.add),
                                      r=[pyb, b_YT], w=[b_YT])
                    kb.op("pool", lambda e: e.tensor_tensor(out=G1, in0=YT[:], in1=YT[:], op=ALU.mult), r=[b_YT], w=[b_G1, all_wk])
                    kb.op("dve", lambda e: e.tensor_scalar(out=G1, in0=G1, scalar1=0.044715, scalar2=1.0, op0=ALU.mult, op1=ALU.add), r=[b_G1], w=[b_G1])
                    kb.op("dve", lambda e: e.tensor_tensor(out=G1, in0=G1, in1=YT[:], op=ALU.mult), r=[b_G1, b_YT], w=[b_G1])
                    kb.op("act", lambda e: e.activation(out=G1, in_=G1, func=AF.Sigmoid, scale=1.5957691216057308), r=[b_G1], w=[b_G1])
                    kb.op("dve", lambda e: e.tensor_tensor(out=ZT[:], in0=G1, in1=YT[:], op=ALU.mult), r=[b_G1, b_YT], w=[b_ZT, all_wk])
                    kb.dma("sp", ZTd[c * 128:(c + 1) * 128, :], ZT[:], r=[b_ZT], w=[Buf("ztd")])
            kb.barrier()

            with contextlib.ExitStack() as st:
                X = P.sb(st, "X", [128, NCH, 512]); bX = Buf("X")
                ZTt = P.sb(st, "ZTt", [128, NCH, 512], BF16); b_ZTt = Buf("ZTt")
                SGt = [P.sb(st, "SGt", [128, 512]) for _ in range(2)]; b_SGt = bufs(2, "SGt")
                NW = 4
                W = [P.sb(st, "W", [128, NCH, 512], BF16) for _ in range(NW)]; b_W = bufs(NW, "W")
                wo = s5_w_out[idx].rearrange("(kc p) n -> p kc n", p=128)
                ZTv = ZTd.rearrange("(c p) t -> p c t", p=128)
                wc = 0
                for (t0, T, s) in tiles:
                    if s == 1 and not with_ctx:
                        continue
                    load_x(X, bX, t0, T)
                    kb.dma("sp", ZTt[:, :, 0:T], ZTv[:, :, t0:t0 + T], w=[b_ZTt])
                    for cb in range(4):
                        ia, ig = wc % NW, (wc + 1) % NW
                        wc += 2
                        P.wget(f"s5o{idx}_a{cb}", W[ia][:], b_W[ia],
                               lambda: kb.dma("pool", W[ia][:], wo[:, :, cb * 512:(cb + 1) * 512], w=[b_W[ia]]))
                        P.wget(f"s5o{idx}_g{cb}", W[ig][:], b_W[ig],
                               lambda: kb.dma("pool", W[ig][:], wo[:, :, D + cb * 512:D + (cb + 1) * 512], w=[b_W[ig]]))
                        for fcn in range(4):
                            m = cb * 4 + fcn
                            pa, pab = P.ps()
                            pg, pgb = P.ps()
                            P.mm_group(pa[:, 0:T], [(W[ia][:, kc, fcn * 128:(fcn + 1) * 128], ZTt[:, kc, 0:T]) for kc in range(NCH)],
                                       r=[b_W[ia], b_ZTt], w=[pab])
                            P.mm_group(pg[:, 0:T], [(W[ig][:, kc, fcn * 128:(fcn + 1) * 128], ZTt[:, kc, 0:T]) for kc in range(NCH)],
                                       r=[b_W[ig], b_ZTt], w=[pgb])
                            i = m % 2
                            kb.op("act", lambda e: e.activation(out=SGt[i][:, 0:T], in_=pg[:, 0:T], func=AF.Sigmoid), r=[pgb], w=[b_SGt[i]])
                            kb.op("dve", lambda e: e.tensor_tensor(out=SGt[i][:, 0:T], in0=pa[:, 0:T], in1=SGt[i][:, 0:T], op=ALU.mult),
                                  r=[pab, b_SGt[i]], w=[b_SGt[i]])
                            kb.op("dve", lambda e: e.scalar_tensor_tensor(out=X[:, m, 0:T], in0=SGt[i][:, 0:T], scalar=Gt[:, 2 + s, m:m + 1],
                                                                          in1=X[:, m, 0:T], op0=ALU.mult, op1=ALU.add),
                                  r=[b_SGt[i], b_G, bX], w=[bX])
                    kb.dma("sp", HTv[:, :, t0:t0 + T], X[:, :, 0:T], r=[bX], w=[tile_buf(t0)])
            kb.barrier()

    for layer in range(L):
        has_next = layer < L - 1
        mods_phase(layer)
        ffn_phase(layer, 0, True)
        kind, idx = layer % 3, layer // 3
        if kind == 0:
            mlstm_layer(idx, has_next)
        elif kind == 1:
            wa_layer(idx, has_next)
        else:
            s5_layer(idx, has_next)
        ffn_phase(layer, 1, has_next)
    final_phase()
    kb.barrier()
    P.es.close()
    return P


def make_inputs(cfg, inputs):
    maps = []
    ident = np.eye(128, dtype=np.float32)
    L = cfg.DEPTH
    shared = {
        "c_ctx": np.ascontiguousarray(inputs["c_ctx"]).reshape(NCH, 128),
        "w_ada": np.ascontiguousarray(inputs["w_ada"][:L]),
        "b_ada": np.ascontiguousarray(inputs["b_ada"][:L]).reshape(L, N_MOD * NCH, 128),
        "g_norm": np.ascontiguousarray(inputs["g_norm"][:L]).reshape(L, 3 * NCH, 128),
        "w_ffn_in": np.ascontiguousarray(inputs["w_ffn_in"][:L]),
        "w_ffn_out": np.ascontiguousarray(inputs["w_ffn_out"][:L]),
        "g_final": np.ascontiguousarray(inputs["g_final"]).reshape(NCH, 128),
        "ident": ident,
        "tri": np.stack([np.triu(np.ones((128, 128), np.float32)), np.tril(np.ones((128, 128), np.float32)),
                         np.ones((128, 128), np.float32)]),
        "ml_w_in": np.ascontiguousarray(inputs["ml_w_in"][:(L + 2) // 3]),
        "ml_b_gate": np.ascontiguousarray(inputs["ml_b_gate"][:(L + 2) // 3]),
        "ml_g_head": np.ascontiguousarray(inputs["ml_g_head"][:(L + 2) // 3]),
        "ml_w_out": np.ascontiguousarray(inputs["ml_w_out"][:(L + 2) // 3]),
    }
    if L // 3 > 0:
        n5 = L // 3
        for k in ("s5_w_in", "s5_lam_re", "s5_lam_im", "s5_log_dt", "s5_b_re", "s5_b_im", "s5_c_re", "s5_c_im", "s5_w_out"):
            shared[k] = np.ascontiguousarray(inputs[k][:n5])
        shared["s5_d_skip"] = np.ascontiguousarray(inputs["s5_d_skip"][:n5]).reshape(n5, NCH, 128)
        shared["iota1"] = np.ascontiguousarray(np.broadcast_to(np.arange(1, 513, dtype=np.float32)[None, :], (128, 512)))
    if (L + 1) // 3 > 0:
        t = np.arange(cfg.SEQ)
        row = (t // 64).astype(np.float32)
        col = (t % 64).astype(np.float32)
        inv = (np.float32(10000.0) ** (-np.arange(32, dtype=np.float32) / np.float32(32))).astype(np.float32)
        ang = np.concatenate([row[:, None] * inv[None, :], col[:, None] * inv[None, :]], axis=-1).astype(np.float32)
        cs, sn = np.cos(ang).astype(np.float32), np.sin(ang).astype(np.float32)
        shared["rope_cos"] = np.ascontiguousarray(np.concatenate([cs, cs], axis=1).T)
        shared["rope_sin"] = np.ascontiguousarray(np.concatenate([-sn, sn], axis=1).T)
        shared["wa_w_in"] = np.ascontiguousarray(inputs["wa_w_in"][:(L + 1) // 3])
        shared["wa_sink"] = np.ascontiguousarray(inputs["wa_sink"][:(L + 1) // 3])
        shared["wa_w_out"] = np.ascontiguousarray(inputs["wa_w_out"][:(L + 1) // 3])
    for b in range(cfg.B):
        m = dict(shared)
        m["x"] = np.ascontiguousarray(inputs["x"][b])
        m["c"] = np.ascontiguousarray(inputs["c"][b]).reshape(NCH, 128)
        m["ctx"] = np.ascontiguousarray(inputs["ctx"][b])
        maps.append(m)
    return maps


def run(cfg, inputs, trace=False):
    P = build(cfg)
    maps = make_inputs(cfg, inputs)
    maps = [{k: v for k, v in m.items() if k in P.din} for m in maps]
    res = run_bass_kernel_spmd(P.nc, maps, core_ids=list(range(cfg.B)), trace=trace)
    outp = np.stack([np.asarray(r["out"]) for r in res.results], axis=0)
    return outp, res


def kernel(**inputs):
    cfg = Cfg()
    outp, _ = run(cfg, inputs)
    return outp.astype(np.float32)
```
